# Optimizing a Trainium2 kernel written in Bass

```python
import jax, jax.numpy as jnp
from jax import lax
import numpy as np

D_MODEL = 1024
BATCH = 16
SEQ = 2048
DEPTH = 4

NORM_EPS = 1e-6
ROPE_THETA = 10000.0
Q_BLOCK = 128

A_HEADS = 8
A_HEAD_DIM = 64
IDX_HEADS = 8
IDX_DIM = 64
DSA_TOPK_MAX = 256

B_HEADS = 8
B_HEAD_DIM = 64
MOBA_BLOCK = 256
MOBA_TOPK = 3
MOBA_Q_CHUNK = 16

EVEN_COLS = (A_HEADS * A_HEAD_DIM,
             A_HEAD_DIM,
             A_HEAD_DIM,
             IDX_HEADS * IDX_DIM,
             IDX_DIM,
             IDX_HEADS,
             B_HEADS * B_HEAD_DIM,
             B_HEADS * B_HEAD_DIM,
             B_HEADS * B_HEAD_DIM)
EVEN_IN_COLS = int(sum(EVEN_COLS))
EVEN_SPLITS = tuple(int(c) for c in np.cumsum(EVEN_COLS)[:-1])
EVEN_OUT_COLS = A_HEADS * A_HEAD_DIM + B_HEADS * B_HEAD_DIM

C_HEADS = 16
C_NOPE = 64
C_ROPE = 32
C_V = 64
C_Q_RANK = 768
C_KV_RANK = 256
C_IN_COLS = C_Q_RANK + C_KV_RANK + C_ROPE

PEER_HEADS = 8
PEER_NKEYS = 128
PEER_N_EXPERTS = PEER_NKEYS * PEER_NKEYS
PEER_QDIM = 256
PEER_TOPK_HALF = 16
PEER_TOPK = 16
PEER_CHUNK = 128

N_EVEN = (DEPTH + 1) // 2
N_ODD = DEPTH // 2

kernel_name = 'hybrid_dsa_moba_mla_peer'


def rms_norm(x, g):
    xf = x.astype(jnp.float32)
    y = xf * lax.rsqrt(jnp.mean(xf * xf, axis=-1, keepdims=True) + NORM_EPS)
    return (y * g.astype(jnp.float32)).astype(x.dtype)


def rope_tables(positions, dim):
    inv = 1.0 / (ROPE_THETA ** (jnp.arange(0, dim, 2, dtype=jnp.float32) / dim))
    ang = positions.astype(jnp.float32)[..., None] * inv
    return jnp.cos(ang)[:, :, None, :], jnp.sin(ang)[:, :, None, :]


def apply_rope(x, cos, sin):
    x1, x2 = jnp.split(x, 2, axis=-1)
    c = cos.astype(x.dtype)
    s = sin.astype(x.dtype)
    return jnp.concatenate([x1 * c - x2 * s, x1 * s + x2 * c], axis=-1)


def to_blocks(a, blk):
    b, s = a.shape[:2]
    return jnp.moveaxis(a.reshape((b, s // blk, blk) + a.shape[2:]), 1, 0)


def from_blocks(a):
    a = jnp.moveaxis(a, 0, 1)
    return a.reshape((a.shape[0], a.shape[1] * a.shape[2]) + a.shape[3:])


def dsa_attention(q, k, v, iq, ik, iw):
    b, s = q.shape[:2]
    n_sel = min(DSA_TOPK_MAX, s // 4)
    key_pos = jnp.arange(s)
    w_scale = (IDX_HEADS ** -0.5) * (IDX_DIM ** -0.5)
    a_scale = A_HEAD_DIM ** -0.5
    gather = jax.vmap(lambda t, i: t[i])

    def one_block(args):
        blk, qb, iqb, iwb = args
        q_pos = blk * Q_BLOCK + jnp.arange(Q_BLOCK)
        causal = key_pos[None, :] <= q_pos[:, None]
        logits = jnp.einsum('bqhd,bsd->bqhs', iqb, ik).astype(jnp.float32)
        score = jnp.einsum('bqhs,bqh->bqs', jax.nn.relu(logits), iwb.astype(jnp.float32)) * w_scale
        score = jnp.where(causal[None], score, -jnp.inf)
        _, sel = lax.top_k(score, n_sel)
        k_sel = gather(k, sel)
        v_sel = gather(v, sel)
        valid = sel <= q_pos[None, :, None]
        sc = jnp.einsum('bqhd,bqnd->bqhn', qb, k_sel).astype(jnp.float32) * a_scale
        sc = jnp.where(valid[:, :, None, :], sc, -jnp.inf)
        p = jax.nn.softmax(sc, axis=-1).astype(v.dtype)
        return jnp.einsum('bqhn,bqnd->bqhd', p, v_sel)

    nblk = s // Q_BLOCK
    out = lax.map(one_block, (jnp.arange(nblk), to_blocks(q, Q_BLOCK),
                              to_blocks(iq, Q_BLOCK), to_blocks(iw, Q_BLOCK)))
    return from_blocks(out)


def moba_attention(q, k, v):
    b, s, h, d = q.shape
    nb = -(-s // MOBA_BLOCK)
    pad = nb * MOBA_BLOCK - s

    def blocks(a):
        a = jnp.pad(a, ((0, 0), (0, pad), (0, 0), (0, 0)))
        return a.reshape(b, nb, MOBA_BLOCK, h, d).transpose(0, 3, 1, 2, 4)

    kb = blocks(k)
    vb = blocks(v)
    k_mean = jnp.mean(kb.astype(jnp.float32), axis=3).astype(k.dtype)
    n_top = min(MOBA_TOPK, nb - 1)
    scale = d ** -0.5
    blk_ids = jnp.arange(nb)
    key_in_blk = jnp.arange(MOBA_BLOCK)
    gather = jax.vmap(jax.vmap(lambda t, i: t[i]))

    def one_chunk(args):
        c, qc = args
        q_pos = c * MOBA_Q_CHUNK + jnp.arange(MOBA_Q_CHUNK)
        own = (c * MOBA_Q_CHUNK) // MOBA_BLOCK
        k_own = lax.dynamic_index_in_dim(kb, own, axis=2, keepdims=False)
        v_own = lax.dynamic_index_in_dim(vb, own, axis=2, keepdims=False)
        s_own = jnp.einsum('bqhd,bhkd->bhqk', qc, k_own).astype(jnp.float32) * scale
        s_own = jnp.where((own * MOBA_BLOCK + key_in_blk)[None, :] <= q_pos[:, None], s_own, -jnp.inf)
        if n_top == 0:
            p = jax.nn.softmax(s_own, axis=-1).astype(v.dtype)
            return jnp.einsum('bhqk,bhkd->bqhd', p, v_own)
        gate = jnp.einsum('bqhd,bhnd->bhqn', qc, k_mean).astype(jnp.float32)
        gate = jnp.where(blk_ids < own, gate, -jnp.inf)
        _, sel = lax.top_k(gate, n_top)
        k_sel = gather(kb, sel)
        v_sel = gather(vb, sel)
        s_past = jnp.einsum('bqhd,bhqnkd->bhqnk', qc, k_sel).astype(jnp.float32) * scale
        s_past = jnp.where((sel < own)[..., None], s_past, -jnp.inf)
        s_past = s_past.reshape(b, h, MOBA_Q_CHUNK, n_top * MOBA_BLOCK)
        p = jax.nn.softmax(jnp.concatenate([s_past, s_own], axis=-1), axis=-1).astype(v.dtype)
        p_past = p[..., :n_top * MOBA_BLOCK].reshape(b, h, MOBA_Q_CHUNK, n_top, MOBA_BLOCK)
        p_own = p[..., n_top * MOBA_BLOCK:]
        return (jnp.einsum('bhqnk,bhqnkd->bqhd', p_past, v_sel)
                + jnp.einsum('bhqk,bhkd->bqhd', p_own, v_own))

    nchunk = s // MOBA_Q_CHUNK
    out = lax.map(one_chunk, (jnp.arange(nchunk), to_blocks(q, MOBA_Q_CHUNK)))
    return from_blocks(out)


def even_mixer(h, cos64, sin64, w_in, w_out):
    b, s, _ = h.shape
    qa, ka, va, iq, ik, iw, qb, kb, vb = jnp.split(h @ w_in, EVEN_SPLITS, axis=-1)
    qa = apply_rope(qa.reshape(b, s, A_HEADS, A_HEAD_DIM), cos64, sin64)
    ka = apply_rope(ka[:, :, None, :], cos64, sin64)[:, :, 0]
    iq = apply_rope(iq.reshape(b, s, IDX_HEADS, IDX_DIM), cos64, sin64)
    ik = apply_rope(ik[:, :, None, :], cos64, sin64)[:, :, 0]
    out_a = dsa_attention(qa, ka, va, iq, ik, iw)
    qb = apply_rope(qb.reshape(b, s, B_HEADS, B_HEAD_DIM), cos64, sin64)
    kb = apply_rope(kb.reshape(b, s, B_HEADS, B_HEAD_DIM), cos64, sin64)
    vb = vb.reshape(b, s, B_HEADS, B_HEAD_DIM)
    out_b = moba_attention(qb, kb, vb)
    o = jnp.concatenate([out_a.reshape(b, s, A_HEADS * A_HEAD_DIM),
                         out_b.reshape(b, s, B_HEADS * B_HEAD_DIM)], axis=-1)
    return o @ w_out


def mla_attention(q_nope, q_rope, k_nope, k_rope, v):
    s = q_nope.shape[1]
    key_pos = jnp.arange(s)
    scale = (C_NOPE + C_ROPE) ** -0.5

    def one_block(args):
        blk, qn, qr = args
        q_pos = blk * Q_BLOCK + jnp.arange(Q_BLOCK)
        sc = (jnp.einsum('bqhd,bshd->bhqs', qn, k_nope)
              + jnp.einsum('bqhd,bsd->bhqs', qr, k_rope)).astype(jnp.float32) * scale
        sc = jnp.where(key_pos[None, :] <= q_pos[:, None], sc, -jnp.inf)
        p = jax.nn.softmax(sc, axis=-1).astype(v.dtype)
        return jnp.einsum('bhqs,bshd->bqhd', p, v)

    nblk = s // Q_BLOCK
    out = lax.map(one_block, (jnp.arange(nblk), to_blocks(q_nope, Q_BLOCK), to_blocks(q_rope, Q_BLOCK)))
    return from_blocks(out)


def mla_mixer(h, cos32, sin32, w_in, q_norm, kv_norm, w_uq, w_ukv, w_out):
    b, s, _ = h.shape
    cq, ckv, kr = jnp.split(h @ w_in, [C_Q_RANK, C_Q_RANK + C_KV_RANK], axis=-1)
    q = (rms_norm(cq, q_norm) @ w_uq).reshape(b, s, C_HEADS, C_NOPE + C_ROPE)
    q_nope, q_rope = jnp.split(q, [C_NOPE], axis=-1)
    q_rope = apply_rope(q_rope, cos32, sin32)
    kv = (rms_norm(ckv, kv_norm) @ w_ukv).reshape(b, s, C_HEADS, C_NOPE + C_V)
    k_nope, v = jnp.split(kv, [C_NOPE], axis=-1)
    k_rope = apply_rope(kr[:, :, None, :], cos32, sin32)[:, :, 0]
    o = mla_attention(q_nope, q_rope, k_nope, k_rope, v)
    return o.reshape(b, s, C_HEADS * C_V) @ w_out


def peer_ffn(h, w_q, sub_keys, u_tab, v_tab):
    b, s, d = h.shape
    tokens = h.reshape(b * s // PEER_CHUNK, PEER_CHUNK, d)

    def one_chunk(xc):
        t = xc.shape[0]
        q = (xc @ w_q).reshape(t, PEER_HEADS, 2, PEER_QDIM // 2)
        sc = jnp.einsum('thpc,hpnc->thpn', q, sub_keys).astype(jnp.float32)
        v_half, i_half = lax.top_k(sc, PEER_TOPK_HALF)
        cand = (v_half[:, :, 0, :, None] + v_half[:, :, 1, None, :]).reshape(t, PEER_HEADS, -1)
        cand_idx = (i_half[:, :, 0, :, None] * PEER_NKEYS + i_half[:, :, 1, None, :]).reshape(t, PEER_HEADS, -1)
        top_s, pos = lax.top_k(cand, PEER_TOPK)
        expert = jnp.take_along_axis(cand_idx, pos, axis=-1)
        g = jax.nn.softmax(top_s, axis=-1)
        u = u_tab[expert]
        act = jax.nn.gelu(jnp.einsum('thkd,td->thk', u, xc).astype(jnp.float32), approximate=False)
        w = (g * act).astype(xc.dtype)
        return jnp.einsum('thk,thkd->td', w, v_tab[expert])

    return lax.map(one_chunk, tokens).reshape(b, s, d)


def setup_inputs(seed: int = 0) -> dict:
    key = jax.random.key(seed)
    ks = jax.random.split(key, 20)
    f32 = jnp.float32

    def nrm(k, shape, scale):
        return jax.random.normal(k, shape, f32) * scale

    def gain(k, shape):
        return 1.0 + 0.02 * jax.random.normal(k, shape, f32)

    return {
        'x': jax.random.normal(ks[0], (BATCH, SEQ, D_MODEL), f32),
        'positions': jnp.broadcast_to(jnp.arange(SEQ, dtype=jnp.int32)[None, :], (BATCH, SEQ)),
        'attn_norm': gain(ks[1], (DEPTH, D_MODEL)),
        'ffn_norm': gain(ks[2], (DEPTH, D_MODEL)),
        'final_norm': gain(ks[3], (D_MODEL,)),
        'hyb_w_in': nrm(ks[4], (N_EVEN, D_MODEL, EVEN_IN_COLS), D_MODEL ** -0.5),
        'hyb_w_out': nrm(ks[5], (N_EVEN, EVEN_OUT_COLS, D_MODEL), EVEN_OUT_COLS ** -0.5),
        'mla_w_in': nrm(ks[6], (N_ODD, D_MODEL, C_IN_COLS), D_MODEL ** -0.5),
        'mla_q_norm': gain(ks[7], (N_ODD, C_Q_RANK)),
        'mla_kv_norm': gain(ks[8], (N_ODD, C_KV_RANK)),
        'mla_w_uq': nrm(ks[9], (N_ODD, C_Q_RANK, C_HEADS * (C_NOPE + C_ROPE)), C_Q_RANK ** -0.5),
        'mla_w_ukv': nrm(ks[10], (N_ODD, C_KV_RANK, C_HEADS * (C_NOPE + C_V)), C_KV_RANK ** -0.5),
        'mla_w_out': nrm(ks[11], (N_ODD, C_HEADS * C_V, D_MODEL), (C_HEADS * C_V) ** -0.5),
        'peer_w_q': nrm(ks[12], (DEPTH, D_MODEL, PEER_HEADS * PEER_QDIM), D_MODEL ** -0.5),
        'peer_sub_keys': nrm(ks[13], (DEPTH, PEER_HEADS, 2, PEER_NKEYS, PEER_QDIM // 2), (PEER_QDIM // 2) ** -0.5),
        'peer_u': nrm(ks[14], (DEPTH, PEER_N_EXPERTS, D_MODEL), D_MODEL ** -0.5),
        'peer_v': nrm(ks[15], (DEPTH, PEER_N_EXPERTS, D_MODEL), D_MODEL ** -0.5),
    }


def reference(x, positions, attn_norm, ffn_norm, final_norm, hyb_w_in, hyb_w_out,
              mla_w_in, mla_q_norm, mla_kv_norm, mla_w_uq, mla_w_ukv, mla_w_out,
              peer_w_q, peer_sub_keys, peer_u, peer_v):
    cos64, sin64 = rope_tables(positions, A_HEAD_DIM)
    cos32, sin32 = rope_tables(positions, C_ROPE)
    for i in range(DEPTH):
        h = rms_norm(x, attn_norm[i])
        j = i // 2
        if i % 2 == 0:
            x = x + even_mixer(h, cos64, sin64, hyb_w_in[j], hyb_w_out[j])
        else:
            x = x + mla_mixer(h, cos32, sin32, mla_w_in[j], mla_q_norm[j], mla_kv_norm[j],
                              mla_w_uq[j], mla_w_ukv[j], mla_w_out[j])
        h = rms_norm(x, ffn_norm[i])
        x = x + peer_ffn(h, peer_w_q[i], peer_sub_keys[i], peer_u[i], peer_v[i])
    return rms_norm(x, final_norm)
```

```python
import math
from contextlib import ExitStack

import numpy as np
import ml_dtypes

import concourse.bass as bass
import concourse.mybir as mybir
from concourse.bass_utils import run_bass_kernel_spmd

F32 = mybir.dt.float32
BF16 = mybir.dt.bfloat16
U32 = mybir.dt.uint32
I32 = mybir.dt.int32
AF = mybir.ActivationFunctionType
ALU = mybir.AluOpType
AX = mybir.AxisListType

NCORES = 8
D = 1024
S = 2048
SPC = 2
NT = S // 128
EPS = 1e-6
NEG = -30000.0
NEGBIG = -1.0e30
SAME_ENGINE_SYNC = True
NP_DMA = 16
PH = {'P1', 'P2', 'P3', 'P3b', 'P4', 'PEER', 'TAB'}
DUMP = False
import os
P3STEP = int(os.environ.get('P3STEP', '99'))
P3VAR = os.environ.get('P3VAR', '')
MSTEP = int(os.environ.get('MSTEP', '99'))
PEERVAR = os.environ.get('PEERVAR', '')

C_ID = 0
C_TRIQK = 128
C_TRIT = 256
C_INV64 = 384
C_SGN64 = 385
C_INV96 = 386
C_SGN96 = 387
C_M1E29 = 388
C_IOTA = 392
NCST = C_IOTA + 256

QA0, KA0, VA0, IQ0, IK0, IW0, QB0, KB0, VB0 = 0, 512, 576, 640, 1152, 1216, 1224, 1736, 2248


def make_consts():
    c = np.zeros((128, NCST), np.float32)
    c[:, C_ID:C_ID + 128] = np.eye(128, dtype=np.float32)
    q = np.arange(128)[:, None]
    k = np.arange(128)[None, :]
    c[:, C_TRIQK:C_TRIQK + 128] = np.where(k <= q, 0.0, NEGBIG)
    c[:, C_TRIT:C_TRIT + 128] = np.where(q <= k, 0.0, NEG)
    p = np.arange(128)
    c[:, C_INV64] = 1.0 / (10000.0 ** ((2.0 * (p % 32)) / 64.0))
    c[:, C_SGN64] = np.where((p % 64) < 32, -1.0, 1.0)
    inv96 = np.zeros(128)
    sgn96 = np.zeros(128)
    for pp in range(64, 96):
        i = (pp - 64) % 16
        inv96[pp] = 1.0 / (10000.0 ** ((2.0 * i) / 32.0))
        sgn96[pp] = -1.0 if (pp - 64) < 16 else 1.0
    c[:, C_INV96] = inv96
    c[:, C_SGN96] = sgn96
    c[:, C_M1E29] = -1.0e29
    c[:, C_IOTA:C_IOTA + 256] = np.arange(256, dtype=np.float32)[None, :]
    return c


class Res:
    __slots__ = ("w", "r", "excl")

    def __init__(self, excl=False):
        self.w = None
        self.r = {}
        self.excl = excl


class V:
    __slots__ = ("ap", "res")

    def __init__(self, ap, res=None):
        self.ap = ap
        self.res = res if res is not None else Res()

    def __getitem__(self, k):
        return V(self.ap[k], self.res)

    def f(self, fn):
        return V(fn(self.ap), self.res)


class Em:
    def __init__(self, nc, es):
        self.nc = nc
        self.es = es
        self.eng = {"pe": nc.tensor, "dve": nc.vector, "act": nc.scalar, "pool": nc.gpsimd, "sp": nc.sync}
        self.sem = {}
        self.tot = {}
        self.seen = {e: {} for e in self.eng}
        for e in self.eng:
            self._mk(e)
        self.dpool = {q: [self._mk("d%s%d" % (q, i)) for i in range(NP_DMA)] for q in ("sp", "pool", "act")}
        self.dk = {q: 0 for q in self.dpool}
        self.ninst = 0

    def _mk(self, name):
        self.sem[name] = self.es.enter_context(self.nc.semaphore(name))
        self.tot[name] = 0
        return name

    def _wait(self, e, name, val):
        if val <= 0 or self.seen[e].get(name, 0) >= val:
            return
        self.eng[e].wait_ge(self.sem[name], val)
        self.seen[e][name] = val

    def _deps(self, e, own, R, W):
        for r in R:
            w = r.res.w
            if w is not None and (w[0] != own or SAME_ENGINE_SYNC):
                self._wait(e, w[0], w[1])
            if r.res.excl:
                for n, v in r.res.r.items():
                    if n != own:
                        self._wait(e, n, v)
        same = SAME_ENGINE_SYNC and e != "pe"
        for x in W:
            w = x.res.w
            if w is not None and (w[0] != own or same):
                self._wait(e, w[0], w[1])
            for n, v in x.res.r.items():
                if n != own or same:
                    self._wait(e, n, v)

    def _commit(self, own, val, R, W):
        for r in R:
            if r.res.r.get(own, 0) < val:
                r.res.r[own] = val
        for x in W:
            x.res.w = (own, val)
            x.res.r = {}

    def op(self, e, fn, R, W):
        self._deps(e, e, R, W)
        inst = fn()
        self.tot[e] += 1
        inst.then_inc(self.sem[e], 1)
        self._commit(e, self.tot[e], R, W)
        self.ninst += 1

    def dma(self, q, out, in_, fn=None, extra_reads=()):
        k = self.dk[q]
        self.dk[q] += 1
        name = self.dpool[q][k % NP_DMA]
        self._wait(q, name, self.tot[name])
        R = [in_] + list(extra_reads)
        self._deps(q, name, R, [out])
        if fn is None:
            inst = self.eng[q].dma_start(out=out.ap, in_=in_.ap)
        else:
            inst = fn()
        self.tot[name] += 16
        inst.then_inc(self.sem[name], 16)
        self._commit(name, self.tot[name], R, [out])
        self.ninst += 1

    def barrier(self):
        for e in self.eng:
            for n, t in self.tot.items():
                if n != e:
                    self._wait(e, n, t)

    def finish(self):
        for n, t in self.tot.items():
            if n != "sp":
                self._wait("sp", n, t)


def build_program(layers, first, last, spc=SPC, dbg=None):
    nc = bass.Bass("TRN2", target_bir_lowering=False)
    ntok = spc * S
    with ExitStack() as es:
        E = Em(nc, es)

        def dr(name, shape, dt, kind="Internal"):
            if kind == "Internal" and DUMP:
                kind = "ExternalOutput"
            return V(nc.dram_tensor(name, list(shape), dt, kind=kind).ap())

        uid = [0]

        def sb(stack, name, shape, dt):
            uid[0] += 1
            t = stack.enter_context(nc.sbuf_tensor("s%d_%s" % (uid[0], name), list(shape), dt))
            return V(t[:])

        x_in = dr("x", [ntok, D], F32, "ExternalInput")
        pos_in = dr("pos", [spc, S], I32, "ExternalInput")
        cst_in = dr("cst", [128, NCST], F32, "ExternalInput")
        y_out = dr("y", [ntok, D], F32, "ExternalOutput")
        anorm = dr("attn_norm", [4, D], F32, "ExternalInput")
        fnorm = dr("ffn_norm", [4, D], F32, "ExternalInput")
        finorm = dr("final_norm", [1, D], F32, "ExternalInput")
        hyb_in = dr("hyb_w_in", [2, D, 2760], F32, "ExternalInput")
        hyb_out = dr("hyb_w_out", [2, D, D], F32, "ExternalInput")
        mla_in = dr("mla_w_in", [2, D, 1056], F32, "ExternalInput")
        mla_gq = dr("mla_gq", [2, 128, 8], F32, "ExternalInput")
        mla_uq = dr("mla_w_uq", [2, 768, 1536], F32, "ExternalInput")
        mla_ukv = dr("mla_w_ukv", [2, 256, 2048], F32, "ExternalInput")
        mla_out = dr("mla_w_out", [2, D, D], F32, "ExternalInput")
        peer_wq = dr("peer_w_q", [4, D, 2048], F32, "ExternalInput")
        peer_sk = dr("peer_sub_keys", [4, 16, 128, 128], F32, "ExternalInput")
        peer_uv = [dr("peer_uv%d" % i, [16384, 2048], F32, "ExternalInput") for i in range(4)]

        uvb = [dr("uvb%d" % i, [16384, 2048], BF16) for i in range(4)]
        xres = y_out if not last else dr("xres", [ntok, D], F32)
        tabC64 = dr("tabC64", [spc, 128, S], F32)
        tabS64 = dr("tabS64", [spc, 128, S], F32)
        tabC96 = dr("tabC96", [spc, 128, S], F32)
        tabS96 = dr("tabS96", [spc, 128, S], F32)
        fmT = dr("fmT", [18, 128, S], BF16)
        va_d = dr("va_d", [S, 64], BF16)
        vb_d = dr("vb_d", [S, 512], BF16)
        iw_d = dr("iw_d", [S, 8], F32)
        oT_d = dr("oT_d", [16, 64, S], BF16)
        qT_d = dr("qT_d", [16, 128, S], BF16)
        kT_d = dr("kT_d", [16, 128, S], BF16)
        V_d = dr("V_d", [S, 1024], BF16)

        G = es
        cst = sb(G, "cst", [128, NCST], F32)
        identb = sb(G, "identb", [128, 128], BF16)
        tritb = sb(G, "tritb", [128, 128], BF16)
        i4b = sb(G, "i4b", [128, 4, 128], BF16)
        ones_b = sb(G, "ones_b", [128, 128], BF16)
        gA = sb(G, "gA", [128, D], F32)
        junk = sb(G, "junk", [128, D], F32)
        st1 = sb(G, "st1", [128, 4], F32)
        ps_all = es.enter_context(nc.psum_tensor("ps_all", [128, 6 * 512], F32))
        psb_all = es.enter_context(nc.psum_tensor("psb_all", [128, 2 * 1024], BF16))
        banks = [V(ps_all[:, i * 512:(i + 1) * 512], Res(True)) for i in range(6)]
        bbanks = [V(psb_all[:, i * 1024:(i + 1) * 1024], Res(True)) for i in range(2)]
        bk = [0, 0]

        rot = [6]

        def psum():
            b = banks[bk[0] % rot[0]]
            bk[0] += 1
            return b

        def psumb():
            b = bbanks[bk[1] % 2]
            bk[1] += 1
            return b

        def mm(out, lhsT, rhs, start=True, stop=True):
            E.op("pe", lambda: nc.tensor.matmul(out.ap, lhsT=lhsT.ap, rhs=rhs.ap, start=start, stop=stop,
                                                skip_group_check=True), [lhsT, rhs], [out])

        def tr(out, in_):
            E.op("pe", lambda: nc.tensor.transpose(out=out.ap, in_=in_.ap, identity=identb.ap), [in_, identb], [out])

        def cp(e, out, in_):
            if e == "act":
                E.op("act", lambda: nc.scalar.copy(out=out.ap, in_=in_.ap), [in_], [out])
            else:
                E.op(e, lambda: E.eng[e].tensor_copy(out=out.ap, in_=in_.ap), [in_], [out])

        def tt(e, out, a, b, op):
            E.op(e, lambda: E.eng[e].tensor_tensor(out=out.ap, in0=a.ap, in1=b.ap, op=op), [a, b], [out])

        def ts(e, out, a, s1, s2, op0, op1=None, extra=()):
            s1a = s1.ap if isinstance(s1, V) else s1
            s2a = s2.ap if isinstance(s2, V) else s2
            R = [a] + [s for s in (s1, s2) if isinstance(s, V)] + list(extra)
            if op1 is None:
                E.op(e, lambda: E.eng[e].tensor_scalar(out=out.ap, in0=a.ap, scalar1=s1a, scalar2=None, op0=op0), R, [out])
            else:
                E.op(e, lambda: E.eng[e].tensor_scalar(out=out.ap, in0=a.ap, scalar1=s1a, scalar2=s2a, op0=op0, op1=op1), R, [out])

        def stt(out, a, s, b, op0, op1, accum=None):
            sa = s.ap if isinstance(s, V) else s
            R = [a, b] + ([s] if isinstance(s, V) else [])
            W = [out] + ([accum] if accum is not None else [])
            if accum is None:
                E.op("dve", lambda: nc.vector.scalar_tensor_tensor(out=out.ap, in0=a.ap, scalar=sa, in1=b.ap, op0=op0, op1=op1), R, W)
            else:
                E.op("dve", lambda: nc.vector.scalar_tensor_tensor(out=out.ap, in0=a.ap, scalar=sa, in1=b.ap, op0=op0, op1=op1,
                                                                   accum_out=accum.ap), R, W)

        def act(out, in_, func, scale=1.0, bias=0.0, accum=None):
            ba = bias.ap if isinstance(bias, V) else bias
            sa = scale.ap if isinstance(scale, V) else scale
            R = [in_] + [s for s in (bias, scale) if isinstance(s, V)]
            W = [out] + ([accum] if accum is not None else [])
            if accum is None:
                E.op("act", lambda: nc.scalar.activation(out=out.ap, in_=in_.ap, func=func, bias=ba, scale=sa), R, W)
            else:
                E.op("act", lambda: nc.scalar.activation(out=out.ap, in_=in_.ap, func=func, bias=ba, scale=sa,
                                                         accum_out=accum.ap), R, W)

        def memset(e, out, val):
            E.op(e, lambda: E.eng[e].memset(out.ap, val), [], [out])

        def recip(out, in_):
            E.op("dve", lambda: nc.vector.reciprocal(out=out.ap, in_=in_.ap), [in_], [out])

        def ld(out, in_, q="sp", slow=False):
            if slow:
                E.dma(q, out, in_, fn=lambda: E.eng[q].dma_start(out=out.ap, in_=in_.ap, allow_slow_non_contiguous=True))
            else:
                E.dma(q, out, in_)

        ld(cst, cst_in)
        cp("dve", identb, cst[:, C_ID:C_ID + 128])
        cp("dve", tritb, cst[:, C_TRIT:C_TRIT + 128])
        for i in range(4):
            cp("dve", i4b[:, i, :], cst[:, C_ID:C_ID + 128])
        memset("dve", ones_b, 1.0)

        if first or True:
            with ExitStack() as ph:
                xb = [sb(ph, "xcp%d" % i, [128, D], F32) for i in range(2)]
                for t in range(ntok // 128):
                    ld(xb[t % 2], x_in[t * 128:(t + 1) * 128, :])
                    E.dma("pool", xres[t * 128:(t + 1) * 128, :], xb[t % 2])
                E.barrier()

        with ExitStack() as ph:
          if 'TAB' in PH:
            pos_i = sb(ph, "pos_i", [128, S], I32)
            pos_f = sb(ph, "pos_f", [128, S], F32)
            u = sb(ph, "tb_u", [128, S], F32)
            ki = sb(ph, "tb_ki", [128, S], I32)
            kf = sb(ph, "tb_kf", [128, S], F32)
            g1 = sb(ph, "tb_g", [128, S], F32)
            m1 = sb(ph, "tb_m", [128, S], F32)
            res_t = sb(ph, "tb_r", [128, S], F32)
            for sq in range(spc):
                E.dma("sp", pos_i, V(pos_in.ap[sq, :].partition_broadcast(128), pos_in.res))
                cp("dve", pos_f, pos_i)
                for (ic, sc_, dC, dS) in ((C_INV64, C_SGN64, tabC64, tabS64), (C_INV96, C_SGN96, tabC96, tabS96)):
                    ts("dve", u, pos_f, cst[:, ic:ic + 1], 1.0 / (2.0 * math.pi), ALU.mult, ALU.mult)
                    cp("dve", ki, u)
                    cp("dve", kf, ki)
                    tt("dve", u, u, kf, ALU.subtract)
                    for (shift, dst, sgn) in ((0.25, dC, None), (0.0, dS, sc_)):
                        ts("dve", g1, u, shift, None, ALU.add)
                        ts("dve", m1, g1, 0.5, None, ALU.is_gt)
                        tt("dve", g1, g1, m1, ALU.subtract)
                        ts("dve", m1, g1, -0.5, None, ALU.is_lt)
                        tt("dve", g1, g1, m1, ALU.add)
                        act(res_t, g1, AF.Sin, scale=2.0 * math.pi)
                        if sgn is not None:
                            ts("dve", res_t, res_t, cst[:, sgn:sgn + 1], None, ALU.mult)
                        E.dma("sp", dst[sq], res_t)
            E.barrier()

        def norm_tile(xt, gt, hb, rstd_tmp, n=D):
            ssq = rstd_tmp[:, 0:1]
            rt = rstd_tmp[:, 1:2]
            rs = rstd_tmp[:, 2:3]
            memset("dve", ssq, 0.0)
            act(junk, xt, AF.Square, accum=ssq)
            act(rt, ssq, AF.Sqrt, scale=1.0 / n, bias=EPS)
            recip(rs, rt)
            stt(hb, xt, rs, gt, ALU.mult, ALU.mult)

        def transpose_to(hb, dstT, col0, nchunks=8):
            pb = psumb()
            for c in range(nchunks):
                tr(pb[:, c * 128:(c + 1) * 128], hb[:, c * 128:(c + 1) * 128])
            cp("act", dstT[:, 0:nchunks, col0:col0 + 128],
               pb[:, 0:nchunks * 128].f(lambda a: a.rearrange("p (c t) -> p c t", t=128)))

        def out_proj_phase(wout_dram, sq):
            if 'P4' not in PH:
                return
            with ExitStack() as ph:
                wst = [sb(ph, "wo_st%d" % i, [64, 16, 256], F32) for i in range(2)]
                wo = sb(ph, "wo_b", [64, 16, D], BF16)
                og = [sb(ph, "og%d" % i, [64, 16, 512], BF16) for i in range(2)]
                xt = [sb(ph, "op_x%d" % i, [128, D], F32) for i in range(2)]
                wv = wout_dram.f(lambda a: a.rearrange("(h d) n -> d h n", d=64))
                for i in range(4):
                    ld(wst[i % 2], wv[:, :, i * 256:(i + 1) * 256])
                    cp("act" if i % 2 else "dve", wo[:, :, i * 256:(i + 1) * 256], wst[i % 2])
                ov = oT_d.f(lambda a: a.rearrange("h d t -> d h t"))
                for tg in range(4):
                    ld(og[tg % 2], ov[:, :, tg * 512:(tg + 1) * 512])
                    for t4 in range(4):
                        tok0 = sq * S + tg * 512 + t4 * 128
                        x_t = xt[t4 % 2]
                        ld(x_t, xres[tok0:tok0 + 128, :])
                        for half in range(2):
                            ps = psum()
                            for h in range(16):
                                mm(ps, og[tg % 2][:, h, t4 * 128:(t4 + 1) * 128], wo[:, h, half * 512:(half + 1) * 512],
                                   start=(h == 0), stop=(h == 15))
                            tt("dve", x_t[:, half * 512:(half + 1) * 512], ps, x_t[:, half * 512:(half + 1) * 512], ALU.add)
                        E.dma("pool", xres[tok0:tok0 + 128, :], x_t)
                E.barrier()

        def pv_norm_store(exps, vfn, M, ncols, dst_fn, tmp_r, tmp_o):
            po = psum()
            pz = psum()
            n = len(exps)
            for i, ex in enumerate(exps):
                mm(po[0:64, 0:ncols], vfn(i), ex, start=(i == 0), stop=(i == n - 1))
            for i, ex in enumerate(exps):
                mm(pz[0:64, 0:ncols], ones_b[:, 0:64], ex, start=(i == 0), stop=(i == n - 1))
            recip(tmp_r[0:64, 0:ncols], pz[0:64, 0:ncols])
            tt("dve", tmp_o[0:64, 0:ncols], po[0:64, 0:ncols], tmp_r[0:64, 0:ncols], ALU.mult)
            dst_fn(tmp_o)

        FM_COLS = [QA0 + 128 * j for j in range(4)] + [IQ0 + 128 * j for j in range(4)] + \
                  [QB0 + 128 * j for j in range(4)] + [KB0 + 128 * j for j in range(4)]
        CH_QA, CH_IQ, CH_QB, CH_KB, CH_KA, CH_IK = 0, 4, 8, 12, 16, 17

        def even_layer(li):
            j = li // 2
            w_in = hyb_in[j]
            with ExitStack() as L:
                sel = sb(L, "sel", [128, 64, 128], BF16)
                E.op("dve", lambda: nc.vector.tensor_copy(
                    out=sel.ap[0:64], in_=identb.ap[0:64, 0:64].unsqueeze(2).to_broadcast([64, 64, 128])), [identb], [sel])
                E.op("dve", lambda: nc.vector.tensor_copy(
                    out=sel.ap[64:128], in_=identb.ap[64:128, 64:128].unsqueeze(2).to_broadcast([64, 64, 128])), [identb], [sel])
                ld(gA, V(anorm.ap[li, :].partition_broadcast(128), anorm.res))
                wv = w_in.f(lambda a: a.rearrange("(c p) n -> p c n", p=128))

                def load_even_weights(stack):
                    Wfm = sb(stack, "Wfm", [128, 8, 18, 128], BF16)
                    Wsw = sb(stack, "Wsw", [128, 8, 18, 128], BF16)
                    Wtok = sb(stack, "Wtok", [128, 8, 584], BF16)
                    with ExitStack() as ph0:
                        stg = [sb(ph0, "wstg%d" % i, [128, 8, 128], F32) for i in range(2)]
                        stk = sb(ph0, "wstk", [128, 8, 584], F32)
                        for ci in range(18):
                            s_ = stg[ci % 2]
                            if ci < 16:
                                ld(s_, wv[:, :, FM_COLS[ci]:FM_COLS[ci] + 128])
                            else:
                                c0 = KA0 if ci == CH_KA else IK0
                                ld(s_[:, :, 0:64], wv[:, :, c0:c0 + 64])
                                ld(s_[:, :, 64:128], wv[:, :, c0:c0 + 64])
                            cp("act", Wfm[:, :, ci, :], s_)
                            sv = s_.f(lambda a: a.rearrange("p c (h t e) -> p c h t e", h=2, t=2))
                            ov = Wsw[:, :, ci, :].f(lambda a: a.rearrange("p c (h t e) -> p c h t e", h=2, t=2))
                            cp("dve", ov[:, :, :, 0, :], sv[:, :, :, 1, :])
                            cp("dve", ov[:, :, :, 1, :], sv[:, :, :, 0, :])
                        ld(stk[:, :, 0:64], wv[:, :, VA0:VA0 + 64])
                        ld(stk[:, :, 64:72], wv[:, :, IW0:IW0 + 8])
                        ld(stk[:, :, 72:584], wv[:, :, VB0:VB0 + 512])
                        cp("act", Wtok, stk)
                        E.barrier()
                    return Wfm, Wsw, Wtok

                for sq in range(spc):
                    with ExitStack() as SQ:
                        km = sb(SQ, "km", [128, 4, 8], F32)
                        kmb = sb(SQ, "kmb", [128, 4, 8], BF16)
                        with ExitStack() as ph:
                          if 'P1' in PH:
                            Wfm, Wsw, Wtok = load_even_weights(ph)
                            Ct = sb(ph, "Ct", [128, S], F32)
                            St = sb(ph, "St", [128, S], F32)
                            ld(Ct, tabC64[sq])
                            ld(St, tabS64[sq])
                            xt = [sb(ph, "p1x%d" % i, [128, D], F32) for i in range(2)]
                            hb = [sb(ph, "p1h%d" % i, [128, D], BF16) for i in range(2)]
                            hT = sb(ph, "hT", [128, 8, 512], BF16)
                            t1 = [sb(ph, "p1t1_%d" % i, [128, 512], F32) for i in range(2)]
                            t2 = [sb(ph, "p1t2_%d" % i, [128, 512], F32) for i in range(2)]
                            o32 = [sb(ph, "p1o32_%d" % i, [128, 512], F32) for i in range(2)]
                            ob = [sb(ph, "p1ob_%d" % i, [128, 512], BF16) for i in range(2)]
                            vat = [sb(ph, "p1va%d" % i, [128, 64], BF16) for i in range(2)]
                            iwt = [sb(ph, "p1iw%d" % i, [128, 8], F32) for i in range(2)]
                            vbt = [sb(ph, "p1vb%d" % i, [128, 512], BF16) for i in range(2)]
                            for tg in range(4):
                                for t4 in range(4):
                                    tok0 = sq * S + tg * 512 + t4 * 128
                                    ld(xt[t4 % 2], xres[tok0:tok0 + 128, :])
                                    norm_tile(xt[t4 % 2], gA, hb[t4 % 2], st1)
                                    transpose_to(hb[t4 % 2], hT, t4 * 128)
                                cs = slice(tg * 512, (tg + 1) * 512)
                                for ci in range(18):
                                    A = psum()
                                    B = psum()
                                    for c in range(8):
                                        mm(A, Wfm[:, c, ci, :], hT[:, c, :], start=(c == 0), stop=(c == 7))
                                    for c in range(8):
                                        mm(B, Wsw[:, c, ci, :], hT[:, c, :], start=(c == 0), stop=(c == 7))
                                    i2 = ci % 2
                                    tt("dve", t1[i2], A, Ct[:, cs], ALU.mult)
                                    tt("dve", t2[i2], B, St[:, cs], ALU.mult)
                                    if CH_KB <= ci < CH_KB + 4:
                                        tt("dve", o32[i2], t1[i2], t2[i2], ALU.add)
                                        E.op("dve", lambda: nc.vector.tensor_reduce(
                                            out=km.ap[:, ci - CH_KB, 2 * tg:2 * tg + 2],
                                            in_=o32[i2].ap.rearrange("p (b k) -> p b k", k=256), axis=AX.X, op=ALU.add),
                                            [o32[i2]], [km])
                                        cp("act", ob[i2], o32[i2])
                                    else:
                                        tt("dve", ob[i2], t1[i2], t2[i2], ALU.add)
                                    E.dma("pool", fmT[ci][:, cs], ob[i2])
                                for t4 in range(4):
                                    r0 = tg * 512 + t4 * 128
                                    p1_ = psum()
                                    p2_ = psum()
                                    for c in range(8):
                                        mm(p1_[:, 0:72], hT[:, c, t4 * 128:(t4 + 1) * 128], Wtok[:, c, 0:72], start=(c == 0), stop=(c == 7))
                                    for c in range(8):
                                        mm(p2_, hT[:, c, t4 * 128:(t4 + 1) * 128], Wtok[:, c, 72:584], start=(c == 0), stop=(c == 7))
                                    i2 = t4 % 2
                                    cp("act", vat[i2], p1_[:, 0:64])
                                    cp("act", iwt[i2], p1_[:, 64:72])
                                    cp("act", vbt[i2], p2_)
                                    E.dma("pool", va_d[r0:r0 + 128, :], vat[i2])
                                    E.dma("pool", iw_d[r0:r0 + 128, :], iwt[i2])
                                    E.dma("pool", vb_d[r0:r0 + 128, :], vbt[i2])
                            ts("dve", kmb, km, 1.0 / 256.0, None, ALU.mult)
                            E.barrier()

                        with ExitStack() as ph:
                          if 'P2' in PH:
                            iq_s = sb(ph, "iq_s", [128, 4, S], BF16)
                            ik_s = sb(ph, "ik_s", [128, S], BF16)
                            qa_s = sb(ph, "qa_s", [128, 4, S], BF16)
                            ka_s = sb(ph, "ka_s", [128, S], BF16)
                            va_s = sb(ph, "va_s", [128, NT, 64], BF16)
                            iw_s = sb(ph, "iw_s", [128, NT, 8], F32)
                            score = sb(ph, "score", [128, S], F32)
                            work = sb(ph, "work", [128, S], F32)
                            rl = [sb(ph, "rl%d" % i, [128, 512], F32) for i in range(2)]
                            biasA = [sb(ph, "biasA%d" % i, [128, S], BF16) for i in range(2)]
                            m8 = sb(ph, "m8", [128, 8], F32)
                            exA = [sb(ph, "exA%d" % i, [128, NT, 512], BF16) for i in range(2)]
                            tr_ = sb(ph, "dsa_r", [64, 512], F32)
                            to_ = [sb(ph, "dsa_o%d" % i, [64, 512], BF16) for i in range(2)]
                            for c in range(4):
                                ld(iq_s[:, c, :], fmT[CH_IQ + c])
                                ld(qa_s[:, c, :], fmT[CH_QA + c])
                            ld(ik_s, fmT[CH_IK])
                            ld(ka_s, fmT[CH_KA])
                            ld(va_s, va_d.f(lambda a: a.rearrange("(t p) d -> p t d", p=128)))
                            ld(iw_s, iw_d.f(lambda a: a.rearrange("(t p) d -> p t d", p=128)))
                            nrl = [0]
                            score2 = [score, sb(ph, "score_b", [128, S], F32)]
                            tmpb = [sb(ph, "ixt%d" % i, [128, 512], F32) for i in range(2)]

                            def indexer(qt):
                                sc_ = score2[qt % 2]
                                nk = (qt + 1) * 128
                                qs = slice(qt * 128, (qt + 1) * 128)
                                for h in range(8):
                                    jj, base = h // 2, 64 * (h % 2)
                                    for kc in range((nk + 511) // 512):
                                        ncol = min(512, nk - kc * 512)
                                        ks = slice(kc * 512, kc * 512 + ncol)
                                        ps = psum()
                                        mm(ps[:, 0:ncol], iq_s[base:base + 64, jj, qs], ik_s[base:base + 64, ks])
                                        r_ = rl[nrl[0] % 2]
                                        t_ = tmpb[nrl[0] % 2]
                                        nrl[0] += 1
                                        act(r_[:, 0:ncol], ps[:, 0:ncol], AF.Relu)
                                        if h == 0:
                                            E.op("act", lambda: nc.scalar.mul(out=sc_.ap[:, ks], in_=r_.ap[:, 0:ncol],
                                                                               mul=iw_s.ap[:, qt, 0:1]), [r_, iw_s], [sc_])
                                        else:
                                            E.op("act", lambda: nc.scalar.mul(out=t_.ap[:, 0:ncol], in_=r_.ap[:, 0:ncol],
                                                                               mul=iw_s.ap[:, qt, h:h + 1]), [r_, iw_s], [t_])
                                            tt("pool", sc_[:, ks], sc_[:, ks], t_[:, 0:ncol], ALU.add)
                                tt("pool", sc_[:, qs], sc_[:, qs], cst[:, C_TRIQK:C_TRIQK + 128], ALU.add)

                            indexer(0)
                            for qt in range(NT):
                                if qt + 1 < NT:
                                    indexer(qt + 1)
                                score = score2[qt % 2]
                                nk = (qt + 1) * 128
                                qs = slice(qt * 128, (qt + 1) * 128)
                                bA = biasA[qt % 2]
                                if qt >= 2:
                                    cur = score
                                    for r in range(32):
                                        E.op("dve", lambda: nc.vector.max(out=m8.ap, in_=cur.ap[:, 0:nk]), [cur], [m8])
                                        if r < 31:
                                            E.op("dve", lambda: nc.vector.match_replace(
                                                out=work.ap[:, 0:nk], in_to_replace=m8.ap, in_values=cur.ap[:, 0:nk],
                                                imm_value=NEGBIG), [m8, cur], [work])
                                            cur = work
                                    thr = m8[:, 7:8]
                                else:
                                    thr = cst[:, C_M1E29:C_M1E29 + 1]
                                ts("dve", bA[:, 0:nk], score[:, 0:nk], thr, NEG, ALU.is_lt, ALU.mult)
                                for hg in range(2):
                                    base = 64 * hg
                                    ex = exA[hg]
                                    for kt in range(qt + 1):
                                        ps = psum()
                                        ps3 = ps.f(lambda a: a.rearrange("p (a b) -> p a b", b=128))
                                        mm(ps3, ka_s[base:base + 64, kt * 128:(kt + 1) * 128], qa_s[base:base + 64, :, qs],
                                           start=True, stop=False)
                                        mm(ps3, bA[:, kt * 128:(kt + 1) * 128], i4b, start=False, stop=True)
                                        act(ex[:, kt, :], ps, AF.Exp, scale=0.125)

                                    def dst(tmp_o, hg=hg, qs=qs):
                                        dv = V(oT_d.ap[0:8].rearrange("(j two) d t -> two j d t", two=2)[hg][:, :, qs].rearrange("j d t -> d j t"), oT_d.res)
                                        E.dma("sp", dv, tmp_o[:, :].f(lambda a: a.rearrange("p (h t) -> p h t", t=128)))
                                    pv_norm_store([ex[:, kt, :] for kt in range(qt + 1)],
                                                  lambda i: va_s[:, i, :], 64, 512, dst, tr_, to_[hg])
                            E.barrier()

                        with ExitStack() as ph:
                          if 'P3' in PH:
                            qb_s = sb(ph, "qb_s", [128, 4, S], BF16)
                            kb_s = sb(ph, "kb_s", [128, 4, S], BF16)
                            vb_s = sb(ph, "vb_s", [128, NT, 512], BF16)
                            bmT = sb(ph, "bmT", [128, S], BF16)
                            gm = sb(ph, "gm", [128, 8, 16], F32)
                            m8b = sb(ph, "m8b", [128, 8, 8], F32)
                            bq = sb(ph, "bq", [128, 8, 8], F32)
                            bqb = sb(ph, "bqb", [128, 128], BF16)
                            memset("dve", bqb, 0.0)
                            exB = [sb(ph, "exB%d" % i, [128, NT, 256], BF16) for i in range(2)]
                            tr_ = sb(ph, "mb_r", [64, 512], F32)
                            to_ = [sb(ph, "mb_o%d" % i, [64, 512], BF16) for i in range(2)]
                            for c in range(4):
                                ld(qb_s[:, c, :], fmT[CH_QB + c])
                                ld(kb_s[:, c, :], fmT[CH_KB + c])
                            ld(vb_s, vb_d.f(lambda a: a.rearrange("(t p) d -> p t d", p=128)))
                            kmpad = sb(ph, "kmpad", [128, 4, 128], BF16)
                            memset("dve", kmpad, 0.0)
                            cp("dve", kmpad[:, :, 0:8], kmb)
                            bqa = sb(ph, "bqa", [128, NT, 128], BF16)
                            memset("dve", bqa, 0.0)
                            if P3VAR == 'zerob':
                                memset("dve", bmT, 0.0)
                            for qt in (range(NT) if P3VAR != 'zerob' else []):
                                own = qt // 2
                                qs = slice(qt * 128, (qt + 1) * 128)
                                if own <= 3:
                                    continue
                                memset("dve", gm, NEGBIG)
                                gmv = gm.f(lambda a: a.rearrange("p (j two) n -> p two j n", two=2))
                                for par in range(2):
                                    ps = psum()
                                    base = 64 * par
                                    for jj in range(4):
                                        mm(ps[:, jj * 128:(jj + 1) * 128], qb_s[base:base + 64, jj, qs], kmpad[base:base + 64, jj, :])
                                    cp("dve", gmv[:, par, :, 0:own],
                                       ps.f(lambda a: a.rearrange("p (h n) -> p h n", n=128))[:, :, 0:own])
                                for h in range(8):
                                    E.op("dve", lambda: nc.vector.max(out=m8b.ap[:, h, :], in_=gm.ap[:, h, :]), [gm], [m8b])
                                E.op("dve", lambda: nc.vector.tensor_tensor(
                                    out=bq.ap, in0=gm.ap[:, :, 0:8], in1=m8b.ap[:, :, 2:3].to_broadcast([128, 8, 8]), op=ALU.is_lt),
                                    [gm, m8b], [bq])
                                ts("dve", bqa[:, qt, 0:64], bq.f(lambda a: a.rearrange("p h n -> p (h n)")), NEG, None, ALU.mult)
                                ts("dve", bqa[:, qt, 64:128], bq.f(lambda a: a.rearrange("p h n -> p (h n)")), NEG, None, ALU.mult)
                            for qt in (range(NT) if P3VAR != 'zerob' else []):
                                qs = slice(qt * 128, (qt + 1) * 128)
                                pt_ = psum()
                                mm(pt_[:, 0:128], bqa[:, qt, :], identb)
                                cp("dve", bmT[:, qs], pt_[:, 0:128])
                            it = 0
                            for h in (range(8) if 'P3b' in PH else []):
                                jj, base = h // 2, 64 * (h % 2)
                                for nb in range(8):
                                    ex = exB[it % 2]
                                    q2 = slice(nb * 256, (nb + 1) * 256)
                                    nkt = 2 * nb + 2
                                    for kt in range(nkt):
                                        n = kt // 2
                                        ksl = slice(kt * 128, (kt + 1) * 128)
                                        ps = psum()
                                        if n < nb:
                                            mm(ps[:, 0:256], kb_s[base:base + 64, jj, ksl], qb_s[base:base + 64, jj, q2], start=True, stop=False)
                                            mm(ps[:, 0:256], sel[base:base + 64, h * 8 + n, :], bmT[base:base + 64, q2], start=False, stop=True)
                                            act(ex[:, kt, :], ps[:, 0:256], AF.Exp, scale=0.125)
                                        elif kt == 2 * nb:
                                            qA = slice(nb * 256, nb * 256 + 128)
                                            qB = slice(nb * 256 + 128, (nb + 1) * 256)
                                            mm(ps[:, 0:128], kb_s[base:base + 64, jj, ksl], qb_s[base:base + 64, jj, qA], start=True, stop=False)
                                            mm(ps[:, 0:128], identb, tritb, start=False, stop=True)
                                            mm(ps[:, 128:256], kb_s[base:base + 64, jj, ksl], qb_s[base:base + 64, jj, qB], start=True, stop=True)
                                            act(ex[:, kt, :], ps[:, 0:256], AF.Exp, scale=0.125)
                                        else:
                                            q1 = slice(nb * 256 + 128, (nb + 1) * 256)
                                            mm(ps[:, 0:128], kb_s[base:base + 64, jj, ksl], qb_s[base:base + 64, jj, q1], start=True, stop=False)
                                            mm(ps[:, 0:128], identb, tritb, start=False, stop=True)
                                            memset("pool", ex[:, kt, 0:128], 0.0)
                                            act(ex[:, kt, 128:256], ps[:, 0:128], AF.Exp, scale=0.125)

                                    def dst(tmp_o, h=h, q2=q2):
                                        E.dma("pool", oT_d[8 + h][:, q2], tmp_o[:, 0:256])
                                    pv_norm_store([ex[:, kt, :] for kt in range(nkt)],
                                                  lambda i, h=h: vb_s[:, i, h * 64:(h + 1) * 64], 64, 256, dst, tr_, to_[it % 2])
                                    it += 1
                            E.barrier()
                    out_proj_phase(hyb_out[j], sq)

        def odd_layer(li):
            j = li // 2
            with ExitStack() as L:
                Win = sb(L, "m_Win", [128, 8, 1024], BF16)
                Wkr = sb(L, "m_Wkr", [128, 8, 128], BF16)
                Wkrs = sb(L, "m_Wkrs", [128, 8, 128], BF16)
                Wuq = sb(L, "m_Wuq", [128, 6, 16, 128], BF16)
                Wuqs = sb(L, "m_Wuqs", [128, 6, 16, 128], BF16)
                Wk = sb(L, "m_Wk", [128, 2, 16, 128], BF16)
                Wv = sb(L, "m_Wv", [128, 2, 16, 64], BF16)
                gq = sb(L, "m_gq", [128, 8], F32)
                ld(gA, V(anorm.ap[li, :].partition_broadcast(128), anorm.res))
                ld(gq, mla_gq[j])
                with ExitStack() as ph:
                    stg = [sb(ph, "mstg%d" % i, [128, 8, 256], F32) for i in range(2)]
                    wv = mla_in[j].f(lambda a: a.rearrange("(c p) n -> p c n", p=128))
                    for i in range(4):
                        ld(stg[i % 2], wv[:, :, i * 256:(i + 1) * 256])
                        cp("act" if i % 2 else "dve", Win[:, :, i * 256:(i + 1) * 256], stg[i % 2])
                    memset("dve", Wkr, 0.0)
                    memset("dve", Wkrs, 0.0)
                    s_ = stg[0]
                    ld(s_[:, :, 0:32], wv[:, :, 1024:1056])
                    cp("dve", Wkr[:, :, 64:96], s_[:, :, 0:32])
                    cp("dve", Wkrs[:, :, 64:80], s_[:, :, 16:32])
                    cp("dve", Wkrs[:, :, 80:96], s_[:, :, 0:16])
                    uv = mla_uq[j].f(lambda a: a.rearrange("(c p) n -> p c n", p=128))
                    memset("dve", Wuq, 0.0)
                    memset("dve", Wuqs, 0.0)
                    stq = sb(ph, "mstq", [128, 6, 768], F32)
                    for i in range(2):
                        ld(stq, uv[:, :, i * 768:(i + 1) * 768])
                        s4 = stq.f(lambda a: a.rearrange("p c (h e) -> p c h e", e=96))
                        cp("act", Wuq[:, :, 8 * i:8 * i + 8, 0:96], s4)
                        cp("dve", Wuqs[:, :, 8 * i:8 * i + 8, 0:64], s4[:, :, :, 0:64])
                        cp("dve", Wuqs[:, :, 8 * i:8 * i + 8, 64:80], s4[:, :, :, 80:96])
                        cp("dve", Wuqs[:, :, 8 * i:8 * i + 8, 80:96], s4[:, :, :, 64:80])
                    kvv = mla_ukv[j].f(lambda a: a.rearrange("(c p) n -> p c n", p=128))
                    memset("dve", Wk, 0.0)
                    for i in range(8):
                        s_ = stg[i % 2][:, 0:2, :]
                        ld(s_, kvv[:, :, i * 256:(i + 1) * 256])
                        s4 = s_.f(lambda a: a.rearrange("p c (h e) -> p c h e", e=128))
                        cp("dve", Wk[:, :, 2 * i:2 * i + 2, 0:64], s4[:, :, :, 0:64])
                        cp("act", Wv[:, :, 2 * i:2 * i + 2, :], s4[:, :, :, 64:128])
                    E.barrier()

                for sq in range(spc):
                    with ExitStack() as ph:
                      if 'P1' in PH:
                        Ct = sb(ph, "mCt", [128, S], F32)
                        St = sb(ph, "mSt", [128, S], F32)
                        ld(Ct, tabC96[sq])
                        ld(St, tabS96[sq])
                        xt = [sb(ph, "m1x%d" % i, [128, D], F32) for i in range(2)]
                        hb = [sb(ph, "m1h%d" % i, [128, D], BF16) for i in range(2)]
                        hT = sb(ph, "mhT", [128, 8, 512], BF16)
                        cT = sb(ph, "mcT", [128, 8, 512], F32)
                        sqb = sb(ph, "msq", [128, 8, 512], BF16)
                        cn = sb(ph, "mcn", [128, 8, 512], BF16)
                        rq = sb(ph, "mrq", [128, 2, 512], F32)
                        kr96 = sb(ph, "mkr96", [128, 512], F32)
                        t1 = [sb(ph, "m1t1_%d" % i, [128, 512], F32) for i in range(2)]
                        t2 = [sb(ph, "m1t2_%d" % i, [128, 512], F32) for i in range(2)]
                        ob = [sb(ph, "m1ob_%d" % i, [128, 512], BF16) for i in range(2)]
                        kb_ = [sb(ph, "m1kb_%d" % i, [128, 512], BF16) for i in range(2)]
                        vt = [sb(ph, "m1vt%d" % i, [128, 1024], BF16) for i in range(2)]
                        for tg in range(4):
                            if MSTEP < 2:
                                continue
                            cs = slice(tg * 512, (tg + 1) * 512)
                            for t4 in range(4):
                                tok0 = sq * S + tg * 512 + t4 * 128
                                ld(xt[t4 % 2], xres[tok0:tok0 + 128, :])
                                norm_tile(xt[t4 % 2], gA, hb[t4 % 2], st1)
                                transpose_to(hb[t4 % 2], hT, t4 * 128)
                            for c in range(8):
                                ps = psum()
                                for d_ in range(8):
                                    mm(ps, Win[:, d_, c * 128:(c + 1) * 128], hT[:, d_, :], start=(d_ == 0), stop=(d_ == 7))
                                cp("dve", cT[:, c, :], ps)
                                act(sqb[:, c, :], ps, AF.Square)
                            if MSTEP < 3:
                                continue
                            for (gi, c0, c1, n_) in ((0, 0, 6, 768), (1, 6, 8, 256)):
                                ps = psum()
                                for c in range(c0, c1):
                                    mm(ps, ones_b, sqb[:, c, :], start=(c == c0), stop=(c == c1 - 1))
                                act(rq[:, gi, :], ps, AF.Sqrt, scale=1.0 / n_, bias=EPS)
                                recip(rq[:, gi, :], rq[:, gi, :])
                                for c in range(c0, c1):
                                    stt(cn[:, c, :], cT[:, c, :], gq[:, c:c + 1], rq[:, gi, :], ALU.mult, ALU.mult)
                            if MSTEP < 4:
                                continue
                            A = psum()
                            B = psum()
                            for d_ in range(8):
                                mm(A, Wkr[:, d_, :], hT[:, d_, :], start=(d_ == 0), stop=(d_ == 7))
                            for d_ in range(8):
                                mm(B, Wkrs[:, d_, :], hT[:, d_, :], start=(d_ == 0), stop=(d_ == 7))
                            tt("dve", t1[0], A, Ct[:, cs], ALU.mult)
                            tt("dve", t2[0], B, St[:, cs], ALU.mult)
                            tt("dve", kr96, t1[0], t2[0], ALU.add)
                            if MSTEP < 5:
                                continue
                            for h in range(16):
                                i2 = h % 2
                                A = psum()
                                B = psum()
                                for c in range(6):
                                    mm(A, Wuq[:, c, h, :], cn[:, c, :], start=(c == 0), stop=(c == 5))
                                for c in range(6):
                                    mm(B, Wuqs[:, c, h, :], cn[:, c, :], start=(c == 0), stop=(c == 5))
                                tt("dve", t1[i2], A, Ct[:, cs], ALU.mult)
                                tt("dve", t2[i2], B, St[:, cs], ALU.mult)
                                tt("dve", ob[i2], t1[i2], t2[i2], ALU.add)
                                E.dma("pool", qT_d[h][:, cs], ob[i2])
                                K = psum()
                                for c in range(2):
                                    mm(K, Wk[:, c, h, :], cn[:, 6 + c, :], start=(c == 0), stop=(c == 1))
                                tt("dve", kb_[i2], K, kr96, ALU.add)
                                E.dma("pool", kT_d[h][:, cs], kb_[i2])
                            if MSTEP < 6:
                                continue
                            for t4 in range(4):
                                r0 = tg * 512 + t4 * 128
                                i2 = t4 % 2
                                for half in range(2):
                                    ps = psum()
                                    for c in range(2):
                                        mm(ps, cn[:, 6 + c, t4 * 128:(t4 + 1) * 128],
                                           Wv[:, c, half * 8:(half + 1) * 8, :].f(lambda a: a.rearrange("p h e -> p (h e)")),
                                           start=(c == 0), stop=(c == 1))
                                    cp("act", vt[i2][:, half * 512:(half + 1) * 512], ps)
                                E.dma("pool", V_d[r0:r0 + 128, :], vt[i2])
                        E.barrier()
                    with ExitStack() as ph:
                      if 'P2' in PH:
                        V_s = sb(ph, "mV_s", [128, NT, 1024], BF16)
                        qh = [sb(ph, "mqh%d" % i, [128, S], BF16) for i in range(2)]
                        kh = [sb(ph, "mkh%d" % i, [128, S], BF16) for i in range(2)]
                        exM = [sb(ph, "exM%d" % i, [128, NT, 512], BF16) for i in range(2)]
                        tr_ = sb(ph, "ml_r", [64, 512], F32)
                        to_ = [sb(ph, "ml_o%d" % i, [64, 512], BF16) for i in range(2)]
                        ld(V_s, V_d.f(lambda a: a.rearrange("(t p) d -> p t d", p=128)))
                        sc_m = 1.0 / math.sqrt(96.0)
                        it = 0
                        for h in range(16):
                            q_h = qh[h % 2]
                            k_h = kh[h % 2]
                            ld(q_h, qT_d[h])
                            ld(k_h, kT_d[h])
                            for qg in range(4):
                                ex = exM[it % 2]
                                nkt = 4 * qg + 4
                                for kt in range(nkt):
                                    lo = max(kt, 4 * qg)
                                    ncol = (4 * qg + 4 - lo) * 128
                                    off = (lo - 4 * qg) * 128
                                    ps = psum()
                                    diag = kt >= 4 * qg
                                    if not diag:
                                        mm(ps[:, 0:ncol], k_h[:, kt * 128:(kt + 1) * 128], q_h[:, lo * 128:(4 * qg + 4) * 128],
                                           start=True, stop=True)
                                    else:
                                        mm(ps[:, 0:128], k_h[:, kt * 128:(kt + 1) * 128], q_h[:, lo * 128:(lo + 1) * 128],
                                           start=True, stop=False)
                                        mm(ps[:, 0:128], identb, tritb, start=False, stop=True)
                                        if ncol > 128:
                                            mm(ps[:, 128:ncol], k_h[:, kt * 128:(kt + 1) * 128],
                                               q_h[:, (lo + 1) * 128:(4 * qg + 4) * 128], start=True, stop=True)
                                        if off > 0:
                                            memset("pool", ex[:, kt, 0:off], 0.0)
                                    act(ex[:, kt, off:512], ps[:, 0:ncol], AF.Exp, scale=sc_m)

                                def dst(tmp_o, h=h, qg=qg):
                                    E.dma("pool", oT_d[h][:, qg * 512:(qg + 1) * 512], tmp_o)
                                pv_norm_store([ex[:, kt, :] for kt in range(nkt)],
                                              lambda i, h=h: V_s[:, i, h * 64:(h + 1) * 64], 64, 512, dst, tr_, to_[it % 2])
                                it += 1
                        E.barrier()
                    out_proj_phase(mla_out[j], sq)

        def peer_layer(li):
            if 'PEER' not in PH:
                return
            with ExitStack() as L:
                Wq = sb(L, "p_Wq", [128, 8, 2048], BF16)
                skT = sb(L, "p_skT", [128, 16, 128], BF16)
                gF = sb(L, "p_gF", [128, D], F32)
                ld(gF, V(fnorm.ap[li, :].partition_broadcast(128), fnorm.res))
                with ExitStack() as ph:
                    stg = [sb(ph, "pstg%d" % i, [128, 8, 256], F32) for i in range(2)]
                    skf = [sb(ph, "pskf%d" % i, [128, 128], F32) for i in range(2)]
                    skb = [sb(ph, "pskb%d" % i, [128, 128], BF16) for i in range(2)]
                    wv = peer_wq[li].f(lambda a: a.rearrange("(c p) n -> p c n", p=128))
                    for i in range(8):
                        ld(stg[i % 2], wv[:, :, i * 256:(i + 1) * 256])
                        cp("act" if i % 2 else "dve", Wq[:, :, i * 256:(i + 1) * 256], stg[i % 2])
                    cin = [sb(ph, "pcin%d" % i, [128, 2, 2048], F32) for i in range(2)]
                    cout = [sb(ph, "pcout%d" % i, [128, 2, 2048], BF16) for i in range(3)]
                    tv = peer_uv[li].f(lambda a: a.rearrange("(p r) c -> p r c", p=128))
                    ov_ = uvb[li].f(lambda a: a.rearrange("(p r) c -> p r c", p=128))
                    engs = ("act", "dve", "pool")
                    for ch in range(64):
                        ld(cin[ch % 2], tv[:, 2 * ch:2 * ch + 2, :])
                        cp(engs[ch % 3], cout[ch % 3], cin[ch % 2])
                        E.dma("sp", ov_[:, 2 * ch:2 * ch + 2, :], cout[ch % 3])
                    for hp in range(16):
                        ld(skf[hp % 2], peer_sk[li][hp])
                        cp("dve", skb[hp % 2], skf[hp % 2])
                        pb = psumb()
                        tr(pb[:, 0:128], skb[hp % 2])
                        cp("act", skT[:, hp, :], pb[:, 0:128])
                    E.barrier()
                with ExitStack() as ph:
                    xt = [sb(ph, "px%d" % i, [128, D], F32) for i in range(2)]
                    h32 = sb(ph, "ph32", [128, D], F32)
                    hb = sb(ph, "phb", [128, D], BF16)
                    hT = sb(ph, "phT", [128, 8, 128], BF16)
                    qT = sb(ph, "pqT", [128, 16, 128], BF16)
                    bufA = sb(ph, "pbufA", [128, 2048], F32)
                    bufB = sb(ph, "pbufB", [128, 2048], F32)
                    sc = bufA.f(lambda a: a.rearrange("p (a b) -> p a b", b=128))
                    scw = bufB.f(lambda a: a.rearrange("p (a b) -> p a b", b=128))
                    vh = sb(ph, "pvh", [128, 16, 16], F32)
                    ih = sb(ph, "pih", [128, 16, 16], U32)
                    ihf = sb(ph, "pihf", [128, 16, 16], F32)
                    cand = bufA.f(lambda a: a.rearrange("p (a b) -> p a b", b=256))
                    candw = bufB.f(lambda a: a.rearrange("p (a b) -> p a b", b=256))
                    cidx = sb(ph, "pcidx", [128, 8, 256], F32)
                    tops = sb(ph, "ptops", [128, 8, 16], F32)
                    posu = sb(ph, "pposu", [128, 8, 16], U32)
                    posf = sb(ph, "pposf", [128, 8, 16], F32)
                    expf = sb(ph, "pexpf", [128, 128], F32)
                    expu = sb(ph, "pexpu", [128, 128], I32)
                    ohj = sb(ph, "pohj", [128, 256], F32)
                    gsm = sb(ph, "pgsm", [128, 8, 16], F32)
                    gsum = sb(ph, "pgsum", [128, 8], F32)
                    actv = sb(ph, "pactv", [128, 128], F32)
                    wgt = sb(ph, "pwgt", [128, 128], F32)
                    NSL = 4
                    NSET = int(os.environ.get('NSET', '4'))
                    gb = [sb(ph, "pgb%d" % i, [128, NSL, 2048], BF16) for i in range(NSET)]
                    dj = sb(ph, "pdj", [128, D], F32)
                    NDG = 4
                    dg = [sb(ph, "pdg%d" % i, [128, 128], BF16) for i in range(NDG)]
                    rot[0] = 4
                    accA, accB = banks[4], banks[5]
                    iota = cst[:, C_IOTA:C_IOTA + 256]
                    gi = [0]
                    h32s = [h32, sb(ph, "ph32b", [128, D], F32)]
                    expus = [expu, sb(ph, "pexpub", [128, 128], I32)]
                    gsms = [gsm, sb(ph, "pgsmb", [128, 8, 16], F32)]

                    def stageA(t):
                        x_t = xt[t % 2]
                        h32 = h32s[t % 2]
                        expu = expus[t % 2]
                        gsm = gsms[t % 2]
                        ld(x_t, xres[t * 128:(t + 1) * 128, :])
                        norm_tile(x_t, gF, h32, st1)
                        cp("act", hb, h32)
                        transpose_to(hb, hT, 0)
                        for g4 in range(4):
                            ps = psum()
                            for k4 in range(4):
                                hp = g4 * 4 + k4
                                for c in range(8):
                                    mm(ps[:, k4 * 128:(k4 + 1) * 128], Wq[:, c, hp * 128:(hp + 1) * 128], hT[:, c, :],
                                       start=(c == 0), stop=(c == 7))
                            cp("act", qT[:, g4 * 4:(g4 + 1) * 4, :], ps.f(lambda a: a.rearrange("p (k t) -> p k t", t=128)))
                        for g4 in range(4):
                            ps = psum()
                            for k4 in range(4):
                                hp = g4 * 4 + k4
                                mm(ps[:, k4 * 128:(k4 + 1) * 128], qT[:, hp, :], skT[:, hp, :])
                            cp("act", sc[:, g4 * 4:(g4 + 1) * 4, :], ps.f(lambda a: a.rearrange("p (k t) -> p k t", t=128)))
                        yield
                        for hp in range(16):
                            E.op("dve", lambda: nc.vector.max(out=vh.ap[:, hp, 0:8], in_=sc.ap[:, hp, :]), [sc], [vh])
                            E.op("dve", lambda: nc.vector.max_index(out=ih.ap[:, hp, 0:8], in_max=vh.ap[:, hp, 0:8],
                                                                    in_values=sc.ap[:, hp, :]), [vh, sc], [ih])
                            E.op("dve", lambda: nc.vector.match_replace(out=scw.ap[:, hp, :], in_to_replace=vh.ap[:, hp, 0:8],
                                                                        in_values=sc.ap[:, hp, :], imm_value=NEGBIG), [vh, sc], [scw])
                        yield
                        for hp in range(16):
                            E.op("dve", lambda: nc.vector.max(out=vh.ap[:, hp, 8:16], in_=scw.ap[:, hp, :]), [scw], [vh])
                            E.op("dve", lambda: nc.vector.max_index(out=ih.ap[:, hp, 8:16], in_max=vh.ap[:, hp, 8:16],
                                                                    in_values=scw.ap[:, hp, :]), [vh, scw], [ih])
                        yield
                        cp("dve", ihf, ih)
                        v4 = vh.f(lambda a: a.rearrange("p (h t) k -> p h t k", t=2))
                        i4 = ihf.f(lambda a: a.rearrange("p (h t) k -> p h t k", t=2))
                        for h in range(8):
                            E.op("dve", lambda: nc.vector.tensor_tensor(
                                out=cand.ap[:, h, :].rearrange("p (a b) -> p a b", b=16),
                                in0=v4.ap[:, h, 0, :].unsqueeze(2).to_broadcast([128, 16, 16]),
                                in1=v4.ap[:, h, 1, :].unsqueeze(1).to_broadcast([128, 16, 16]), op=ALU.add), [vh], [cand])
                            E.op("dve", lambda: nc.vector.scalar_tensor_tensor(
                                out=cidx.ap[:, h, :].rearrange("p (a b) -> p a b", b=16),
                                in0=i4.ap[:, h, 0, :].unsqueeze(2).to_broadcast([128, 16, 16]), scalar=128.0,
                                in1=i4.ap[:, h, 1, :].unsqueeze(1).to_broadcast([128, 16, 16]),
                                op0=ALU.mult, op1=ALU.add), [ihf], [cidx])
                        yield
                        for h in range(8):
                            E.op("dve", lambda: nc.vector.max(out=tops.ap[:, h, 0:8], in_=cand.ap[:, h, :]), [cand], [tops])
                            E.op("dve", lambda: nc.vector.max_index(out=posu.ap[:, h, 0:8], in_max=tops.ap[:, h, 0:8],
                                                                    in_values=cand.ap[:, h, :]), [tops, cand], [posu])
                            E.op("dve", lambda: nc.vector.match_replace(out=candw.ap[:, h, :], in_to_replace=tops.ap[:, h, 0:8],
                                                                        in_values=cand.ap[:, h, :], imm_value=NEGBIG), [tops, cand], [candw])
                        yield
                        for h in range(8):
                            E.op("dve", lambda: nc.vector.max(out=tops.ap[:, h, 8:16], in_=candw.ap[:, h, :]), [candw], [tops])
                            E.op("dve", lambda: nc.vector.max_index(out=posu.ap[:, h, 8:16], in_max=tops.ap[:, h, 8:16],
                                                                    in_values=candw.ap[:, h, :]), [tops, candw], [posu])
                        yield
                        cp("dve", posf, posu)
                        memset("dve", expf, 0.0)
                        for h in range(8):
                            yield
                            for k in range(16):
                                stt(ohj, iota, posf[:, h, k:k + 1], cidx[:, h, :], ALU.is_equal, ALU.mult,
                                    accum=expf[:, h * 16 + k:h * 16 + k + 1])
                        yield
                        cp("dve", expu, expf)
                        E.op("dve", lambda: nc.vector.tensor_tensor(
                            out=gsm.ap, in0=tops.ap, in1=tops.ap[:, :, 0:1].to_broadcast([128, 8, 16]), op=ALU.subtract), [tops], [gsm])
                        act(gsm, gsm, AF.Exp)
                        E.op("dve", lambda: nc.vector.tensor_reduce(out=gsum.ap, in_=gsm.ap, axis=AX.X, op=ALU.add), [gsm], [gsum])
                        recip(gsum, gsum)
                        E.op("dve", lambda: nc.vector.tensor_tensor(
                            out=gsm.ap, in0=gsm.ap, in1=gsum.ap.unsqueeze(2).to_broadcast([128, 8, 16]), op=ALU.mult), [gsm, gsum], [gsm])

                    def stageB(t):
                        x_t = xt[t % 2]
                        h32 = h32s[t % 2]
                        expu = expus[t % 2]
                        gsm = gsms[t % 2]
                        gflat = gsm.f(lambda a: a.rearrange("p h k -> p (h k)"))
                        memset("dve", actv, 0.0)
                        first_acc = True
                        for g_ in range(128 // NSL):
                            if genA[0] is not None and g_ % 2 == 0:
                                next(genA[0], None)
                            buf = gb[gi[0] % NSET]
                            gi[0] += 1
                            for s_ in (range(NSL) if PEERVAR != 'nogather' else []):
                                sl = g_ * NSL + s_
                                E.dma("pool", buf[:, s_, :], uvb[li],
                                      fn=(lambda: nc.gpsimd.indirect_dma_start(
                                          out=buf.ap[:, s_, 0:1024], out_offset=None, in_=peer_uv[li].ap[:, 0:1024],
                                          in_offset=bass.IndirectOffsetOnAxis(ap=expu.ap[:, sl:sl + 1], axis=0))) if PEERVAR == 'half' else lambda: nc.gpsimd.indirect_dma_start(
                                          out=buf.ap[:, s_, :], out_offset=None, in_=uvb[li].ap,
                                          in_offset=bass.IndirectOffsetOnAxis(ap=expu.ap[:, sl:sl + 1], axis=0)),
                                      extra_reads=[expu])
                            if PEERVAR in ('nocompute', 'half'):
                                continue
                            for s_ in range(NSL):
                                sl = g_ * NSL + s_
                                stt(dj, buf[:, s_, 0:D], 1.0, h32, ALU.mult, ALU.mult, accum=actv[:, sl:sl + 1])
                            c0 = g_ * NSL
                            act(wgt[:, c0:c0 + NSL], actv[:, c0:c0 + NSL], AF.Gelu)
                            tt("dve", wgt[:, c0:c0 + NSL], wgt[:, c0:c0 + NSL], gflat[:, c0:c0 + NSL], ALU.mult)
                            for s_ in range(NSL):
                                sl = g_ * NSL + s_
                                d_ = dg[sl % NDG]
                                ts("dve", d_, identb, wgt[:, sl:sl + 1], None, ALU.mult)
                                mm(accA, d_, buf[:, s_, D:D + 512], start=(sl == 0), stop=(sl == 127))
                                mm(accB, d_, buf[:, s_, D + 512:2 * D], start=(sl == 0), stop=(sl == 127))
                        tt("dve", x_t[:, 0:512], accA, x_t[:, 0:512], ALU.add)
                        tt("dve", x_t[:, 512:1024], accB, x_t[:, 512:1024], ALU.add)
                        E.dma("sp", xres[t * 128:(t + 1) * 128, :], x_t)

                    ntile = ntok // 128
                    genA = [None]
                    for _ in stageA(0):
                        pass
                    for t in range(ntile):
                        genA[0] = stageA(t + 1) if t + 1 < ntile else None
                        stageB(t)
                        if genA[0] is not None:
                            for _ in genA[0]:
                                pass
                    E.barrier()
                    rot[0] = 6

        for li in layers:
            if li % 2 == 0:
                even_layer(li)
            else:
                odd_layer(li)
            peer_layer(li)

        if last:
            with ExitStack() as ph:
                xt = [sb(ph, "fx%d" % i, [128, D], F32) for i in range(2)]
                yo = [sb(ph, "fy%d" % i, [128, D], F32) for i in range(2)]
                ld(gA, V(finorm.ap[0, :].partition_broadcast(128), finorm.res))
                for t in range(ntok // 128):
                    ld(xt[t % 2], xres[t * 128:(t + 1) * 128, :])
                    norm_tile(xt[t % 2], gA, yo[t % 2], st1)
                    E.dma("sp", y_out[t * 128:(t + 1) * 128, :], yo[t % 2])
        E.barrier()
        E.finish()
        print("built program: %d instructions" % E.ninst, flush=True)
    return nc


def mla_gq_layout(qn, kvn):
    qn = np.asarray(qn, dtype=np.float32).reshape(2, 6, 128)
    kvn = np.asarray(kvn, dtype=np.float32).reshape(2, 2, 128)
    return np.ascontiguousarray(np.concatenate([qn, kvn], axis=1).transpose(0, 2, 1))


_CACHE = {}


def _get_prog(key, *a, **k):
    if key not in _CACHE:
        _CACHE[key] = build_program(*a, **k)
    return _CACHE[key]


def kernel(x, positions, attn_norm, ffn_norm, final_norm, hyb_w_in, hyb_w_out,
           mla_w_in, mla_q_norm, mla_kv_norm, mla_w_uq, mla_w_ukv, mla_w_out,
           peer_w_q, peer_sub_keys, peer_u, peer_v):
    f = lambda a: np.ascontiguousarray(np.asarray(a, dtype=np.float32))
    x = f(x).reshape(NCORES, SPC * S, D)
    pos = np.ascontiguousarray(np.asarray(positions, dtype=np.int32)).reshape(NCORES, SPC, S)
    peer_uv = np.concatenate([f(peer_u), f(peer_v)], axis=-1)
    common = {
        "cst": make_consts(),
        "attn_norm": f(attn_norm), "ffn_norm": f(ffn_norm), "final_norm": f(final_norm).reshape(1, D),
        "hyb_w_in": f(hyb_w_in), "hyb_w_out": f(hyb_w_out), "mla_w_in": f(mla_w_in),
        "mla_gq": mla_gq_layout(mla_q_norm, mla_kv_norm), "mla_w_uq": f(mla_w_uq),
        "mla_w_ukv": f(mla_w_ukv), "mla_w_out": f(mla_w_out), "peer_w_q": f(peer_w_q),
        "peer_sub_keys": f(peer_sub_keys).reshape(4, 16, 128, 128),
    }
    for i in range(4):
        common["peer_uv%d" % i] = peer_uv[i]
    nc = _get_prog("full", [0, 1, 2, 3], True, True)
    in_maps = []
    for c in range(NCORES):
        m = dict(common)
        m["x"] = x[c]
        m["pos"] = pos[c]
        in_maps.append(m)
    res = run_bass_kernel_spmd(nc, in_maps, core_ids=list(range(NCORES)))
    out = np.stack([np.asarray(r["y"]) for r in res.results], axis=0)
    return out.reshape(16, S, D).astype(np.float32)
```

```python
import math
from contextlib import ExitStack

import numpy as np
import ml_dtypes

import concourse.bass as bass
import concourse.mybir as mybir
from concourse.bass_utils import run_bass_kernel_spmd

F32 = mybir.dt.float32
BF16 = mybir.dt.bfloat16
U32 = mybir.dt.uint32
I32 = mybir.dt.int32
AF = mybir.ActivationFunctionType
ALU = mybir.AluOpType
AX = mybir.AxisListType

NCORES = 8
D = 1024
S = 2048
SPC = 2
NT = S // 128
EPS = 1e-6
NEG = -30000.0
NEGBIG = -1.0e30
SAME_ENGINE_SYNC = True
NP_DMA = 16
PH = {'P1', 'P2', 'P3', 'P3b', 'P4', 'PEER', 'TAB'}
DUMP = False
import os
P3STEP = int(os.environ.get('P3STEP', '99'))
P3VAR = os.environ.get('P3VAR', '')
MSTEP = int(os.environ.get('MSTEP', '99'))
PEERVAR = os.environ.get('PEERVAR', '')

C_ID = 0
C_TRIQK = 128
C_TRIT = 256
C_INV64 = 384
C_SGN64 = 385
C_INV96 = 386
C_SGN96 = 387
C_M1E29 = 388
C_IOTA = 392
NCST = C_IOTA + 256

QA0, KA0, VA0, IQ0, IK0, IW0, QB0, KB0, VB0 = 0, 512, 576, 640, 1152, 1216, 1224, 1736, 2248


def make_consts():
    c = np.zeros((128, NCST), np.float32)
    c[:, C_ID:C_ID + 128] = np.eye(128, dtype=np.float32)
    q = np.arange(128)[:, None]
    k = np.arange(128)[None, :]
    c[:, C_TRIQK:C_TRIQK + 128] = np.where(k <= q, 0.0, NEGBIG)
    c[:, C_TRIT:C_TRIT + 128] = np.where(q <= k, 0.0, NEG)
    p = np.arange(128)
    c[:, C_INV64] = 1.0 / (10000.0 ** ((2.0 * (p % 32)) / 64.0))
    c[:, C_SGN64] = np.where((p % 64) < 32, -1.0, 1.0)
    inv96 = np.zeros(128)
    sgn96 = np.zeros(128)
    for pp in range(64, 96):
        i = (pp - 64) % 16
        inv96[pp] = 1.0 / (10000.0 ** ((2.0 * i) / 32.0))
        sgn96[pp] = -1.0 if (pp - 64) < 16 else 1.0
    c[:, C_INV96] = inv96
    c[:, C_SGN96] = sgn96
    c[:, C_M1E29] = -1.0e29
    c[:, C_IOTA:C_IOTA + 256] = np.arange(256, dtype=np.float32)[None, :]
    return c


class Res:
    __slots__ = ("w", "r", "excl")

    def __init__(self, excl=False):
        self.w = None
        self.r = {}
        self.excl = excl


class V:
    __slots__ = ("ap", "res")

    def __init__(self, ap, res=None):
        self.ap = ap
        self.res = res if res is not None else Res()

    def __getitem__(self, k):
        return V(self.ap[k], self.res)

    def f(self, fn):
        return V(fn(self.ap), self.res)


class Em:
    def __init__(self, nc, es):
        self.nc = nc
        self.es = es
        self.eng = {"pe": nc.tensor, "dve": nc.vector, "act": nc.scalar, "pool": nc.gpsimd, "sp": nc.sync}
        self.sem = {}
        self.tot = {}
        self.seen = {e: {} for e in self.eng}
        for e in self.eng:
            self._mk(e)
        self.dpool = {q: [self._mk("d%s%d" % (q, i)) for i in range(NP_DMA)] for q in ("sp", "pool", "act")}
        self.dk = {q: 0 for q in self.dpool}
        self.ninst = 0

    def _mk(self, name):
        self.sem[name] = self.es.enter_context(self.nc.semaphore(name))
        self.tot[name] = 0
        return name

    def _wait(self, e, name, val):
        if val <= 0 or self.seen[e].get(name, 0) >= val:
            return
        self.eng[e].wait_ge(self.sem[name], val)
        self.seen[e][name] = val

    def _deps(self, e, own, R, W):
        for r in R:
            w = r.res.w
            if w is not None and (w[0] != own or SAME_ENGINE_SYNC):
                self._wait(e, w[0], w[1])
            if r.res.excl:
                for n, v in r.res.r.items():
                    if n != own:
                        self._wait(e, n, v)
        same = SAME_ENGINE_SYNC and e != "pe"
        for x in W:
            w = x.res.w
            if w is not None and (w[0] != own or same):
                self._wait(e, w[0], w[1])
            for n, v in x.res.r.items():
                if n != own or same:
                    self._wait(e, n, v)

    def _commit(self, own, val, R, W):
        for r in R:
            if r.res.r.get(own, 0) < val:
                r.res.r[own] = val
        for x in W:
            x.res.w = (own, val)
            x.res.r = {}

    def op(self, e, fn, R, W):
        self._deps(e, e, R, W)
        inst = fn()
        self.tot[e] += 1
        inst.then_inc(self.sem[e], 1)
        self._commit(e, self.tot[e], R, W)
        self.ninst += 1

    def dma(self, q, out, in_, fn=None, extra_reads=()):
        k = self.dk[q]
        self.dk[q] += 1
        name = self.dpool[q][k % NP_DMA]
        self._wait(q, name, self.tot[name])
        R = [in_] + list(extra_reads)
        self._deps(q, name, R, [out])
        if fn is None:
            inst = self.eng[q].dma_start(out=out.ap, in_=in_.ap)
        else:
            inst = fn()
        self.tot[name] += 16
        inst.then_inc(self.sem[name], 16)
        self._commit(name, self.tot[name], R, [out])
        self.ninst += 1

    def barrier(self):
        for e in self.eng:
            for n, t in self.tot.items():
                if n != e:
                    self._wait(e, n, t)

    def finish(self):
        for n, t in self.tot.items():
            if n != "sp":
                self._wait("sp", n, t)


def build_program(layers, first, last, spc=SPC, dbg=None):
    nc = bass.Bass("TRN2", target_bir_lowering=False)
    ntok = spc * S
    with ExitStack() as es:
        E = Em(nc, es)

        def dr(name, shape, dt, kind="Internal"):
            if kind == "Internal" and DUMP:
                kind = "ExternalOutput"
            return V(nc.dram_tensor(name, list(shape), dt, kind=kind).ap())

        uid = [0]

        def sb(stack, name, shape, dt):
            uid[0] += 1
            t = stack.enter_context(nc.sbuf_tensor("s%d_%s" % (uid[0], name), list(shape), dt))
            return V(t[:])

        x_in = dr("x", [ntok, D], F32, "ExternalInput")
        pos_in = dr("pos", [spc, S], I32, "ExternalInput")
        cst_in = dr("cst", [128, NCST], F32, "ExternalInput")
        y_out = dr("y", [ntok, D], F32, "ExternalOutput")
        anorm = dr("attn_norm", [4, D], F32, "ExternalInput")
        fnorm = dr("ffn_norm", [4, D], F32, "ExternalInput")
        finorm = dr("final_norm", [1, D], F32, "ExternalInput")
        hyb_in = dr("hyb_w_in", [2, D, 2760], F32, "ExternalInput")
        hyb_out = dr("hyb_w_out", [2, D, D], F32, "ExternalInput")
        mla_in = dr("mla_w_in", [2, D, 1056], F32, "ExternalInput")
        mla_gq = dr("mla_gq", [2, 128, 8], F32, "ExternalInput")
        mla_uq = dr("mla_w_uq", [2, 768, 1536], F32, "ExternalInput")
        mla_ukv = dr("mla_w_ukv", [2, 256, 2048], F32, "ExternalInput")
        mla_out = dr("mla_w_out", [2, D, D], F32, "ExternalInput")
        peer_wq = dr("peer_w_q", [4, D, 2048], F32, "ExternalInput")
        peer_sk = dr("peer_sub_keys", [4, 16, 128, 128], F32, "ExternalInput")
        peer_uv = [dr("peer_uv%d" % i, [16384, 2048], F32, "ExternalInput") for i in range(4)]

        uvb = [dr("uvb%d" % i, [16384, 2048], BF16) for i in range(4)]
        xres = y_out if not last else dr("xres", [ntok, D], F32)
        tabC64 = dr("tabC64", [spc, 128, S], F32)
        tabS64 = dr("tabS64", [spc, 128, S], F32)
        tabC96 = dr("tabC96", [spc, 128, S], F32)
        tabS96 = dr("tabS96", [spc, 128, S], F32)
        fmT = dr("fmT", [18, 128, S], BF16)
        va_d = dr("va_d", [S, 64], BF16)
        vb_d = dr("vb_d", [S, 512], BF16)
        iw_d = dr("iw_d", [S, 8], F32)
        oT_d = dr("oT_d", [16, 64, S], BF16)
        qT_d = dr("qT_d", [16, 128, S], BF16)
        kT_d = dr("kT_d", [16, 128, S], BF16)
        V_d = dr("V_d", [S, 1024], BF16)

        G = es
        cst = sb(G, "cst", [128, NCST], F32)
        identb = sb(G, "identb", [128, 128], BF16)
        tritb = sb(G, "tritb", [128, 128], BF16)
        i4b = sb(G, "i4b", [128, 4, 128], BF16)
        ones_b = sb(G, "ones_b", [128, 128], BF16)
        gA = sb(G, "gA", [128, D], F32)
        junk = sb(G, "junk", [128, D], F32)
        st1 = sb(G, "st1", [128, 4], F32)
        ps_all = es.enter_context(nc.psum_tensor("ps_all", [128, 6 * 512], F32))
        psb_all = es.enter_context(nc.psum_tensor("psb_all", [128, 2 * 1024], BF16))
        banks = [V(ps_all[:, i * 512:(i + 1) * 512], Res(True)) for i in range(6)]
        bbanks = [V(psb_all[:, i * 1024:(i + 1) * 1024], Res(True)) for i in range(2)]
        bk = [0, 0]

        rot = [6]

        def psum():
            b = banks[bk[0] % rot[0]]
            bk[0] += 1
            return b

        def psumb():
            b = bbanks[bk[1] % 2]
            bk[1] += 1
            return b

        def mm(out, lhsT, rhs, start=True, stop=True):
            E.op("pe", lambda: nc.tensor.matmul(out.ap, lhsT=lhsT.ap, rhs=rhs.ap, start=start, stop=stop,
                                                skip_group_check=True), [lhsT, rhs], [out])

        def tr(out, in_):
            E.op("pe", lambda: nc.tensor.transpose(out=out.ap, in_=in_.ap, identity=identb.ap), [in_, identb], [out])

        def cp(e, out, in_):
            if e == "act":
                E.op("act", lambda: nc.scalar.copy(out=out.ap, in_=in_.ap), [in_], [out])
            else:
                E.op(e, lambda: E.eng[e].tensor_copy(out=out.ap, in_=in_.ap), [in_], [out])

        def tt(e, out, a, b, op):
            E.op(e, lambda: E.eng[e].tensor_tensor(out=out.ap, in0=a.ap, in1=b.ap, op=op), [a, b], [out])

        def ts(e, out, a, s1, s2, op0, op1=None, extra=()):
            s1a = s1.ap if isinstance(s1, V) else s1
            s2a = s2.ap if isinstance(s2, V) else s2
            R = [a] + [s for s in (s1, s2) if isinstance(s, V)] + list(extra)
            if op1 is None:
                E.op(e, lambda: E.eng[e].tensor_scalar(out=out.ap, in0=a.ap, scalar1=s1a, scalar2=None, op0=op0), R, [out])
            else:
                E.op(e, lambda: E.eng[e].tensor_scalar(out=out.ap, in0=a.ap, scalar1=s1a, scalar2=s2a, op0=op0, op1=op1), R, [out])

        def stt(out, a, s, b, op0, op1, accum=None):
            sa = s.ap if isinstance(s, V) else s
            R = [a, b] + ([s] if isinstance(s, V) else [])
            W = [out] + ([accum] if accum is not None else [])
            if accum is None:
                E.op("dve", lambda: nc.vector.scalar_tensor_tensor(out=out.ap, in0=a.ap, scalar=sa, in1=b.ap, op0=op0, op1=op1), R, W)
            else:
                E.op("dve", lambda: nc.vector.scalar_tensor_tensor(out=out.ap, in0=a.ap, scalar=sa, in1=b.ap, op0=op0, op1=op1,
                                                                   accum_out=accum.ap), R, W)

        def act(out, in_, func, scale=1.0, bias=0.0, accum=None):
            ba = bias.ap if isinstance(bias, V) else bias
            sa = scale.ap if isinstance(scale, V) else scale
            R = [in_] + [s for s in (bias, scale) if isinstance(s, V)]
            W = [out] + ([accum] if accum is not None else [])
            if accum is None:
                E.op("act", lambda: nc.scalar.activation(out=out.ap, in_=in_.ap, func=func, bias=ba, scale=sa), R, W)
            else:
                E.op("act", lambda: nc.scalar.activation(out=out.ap, in_=in_.ap, func=func, bias=ba, scale=sa,
                                                         accum_out=accum.ap), R, W)

        def memset(e, out, val):
            E.op(e, lambda: E.eng[e].memset(out.ap, val), [], [out])

        def recip(out, in_):
            E.op("dve", lambda: nc.vector.reciprocal(out=out.ap, in_=in_.ap), [in_], [out])

        def ld(out, in_, q="sp", slow=False):
            if slow:
                E.dma(q, out, in_, fn=lambda: E.eng[q].dma_start(out=out.ap, in_=in_.ap, allow_slow_non_contiguous=True))
            else:
                E.dma(q, out, in_)

        ld(cst, cst_in)
        cp("dve", identb, cst[:, C_ID:C_ID + 128])
        cp("dve", tritb, cst[:, C_TRIT:C_TRIT + 128])
        for i in range(4):
            cp("dve", i4b[:, i, :], cst[:, C_ID:C_ID + 128])
        memset("dve", ones_b, 1.0)

        if first or True:
            with ExitStack() as ph:
                xb = [sb(ph, "xcp%d" % i, [128, D], F32) for i in range(2)]
                for t in range(ntok // 128):
                    ld(xb[t % 2], x_in[t * 128:(t + 1) * 128, :])
                    E.dma("pool", xres[t * 128:(t + 1) * 128, :], xb[t % 2])
                E.barrier()

        with ExitStack() as ph:
          if 'TAB' in PH:
            pos_i = sb(ph, "pos_i", [128, S], I32)
            pos_f = sb(ph, "pos_f", [128, S], F32)
            u = sb(ph, "tb_u", [128, S], F32)
            ki = sb(ph, "tb_ki", [128, S], I32)
            kf = sb(ph, "tb_kf", [128, S], F32)
            g1 = sb(ph, "tb_g", [128, S], F32)
            m1 = sb(ph, "tb_m", [128, S], F32)
            res_t = sb(ph, "tb_r", [128, S], F32)
            for sq in range(spc):
                E.dma("sp", pos_i, V(pos_in.ap[sq, :].partition_broadcast(128), pos_in.res))
                cp("dve", pos_f, pos_i)
                for (ic, sc_, dC, dS) in ((C_INV64, C_SGN64, tabC64, tabS64), (C_INV96, C_SGN96, tabC96, tabS96)):
                    ts("dve", u, pos_f, cst[:, ic:ic + 1], 1.0 / (2.0 * math.pi), ALU.mult, ALU.mult)
                    cp("dve", ki, u)
                    cp("dve", kf, ki)
                    tt("dve", u, u, kf, ALU.subtract)
                    for (shift, dst, sgn) in ((0.25, dC, None), (0.0, dS, sc_)):
                        ts("dve", g1, u, shift, None, ALU.add)
                        ts("dve", m1, g1, 0.5, None, ALU.is_gt)
                        tt("dve", g1, g1, m1, ALU.subtract)
                        ts("dve", m1, g1, -0.5, None, ALU.is_lt)
                        tt("dve", g1, g1, m1, ALU.add)
                        act(res_t, g1, AF.Sin, scale=2.0 * math.pi)
                        if sgn is not None:
                            ts("dve", res_t, res_t, cst[:, sgn:sgn + 1], None, ALU.mult)
                        E.dma("sp", dst[sq], res_t)
            E.barrier()

        def norm_tile(xt, gt, hb, rstd_tmp, n=D):
            ssq = rstd_tmp[:, 0:1]
            rt = rstd_tmp[:, 1:2]
            rs = rstd_tmp[:, 2:3]
            memset("dve", ssq, 0.0)
            act(junk, xt, AF.Square, accum=ssq)
            act(rt, ssq, AF.Sqrt, scale=1.0 / n, bias=EPS)
            recip(rs, rt)
            stt(hb, xt, rs, gt, ALU.mult, ALU.mult)

        def transpose_to(hb, dstT, col0, nchunks=8):
            pb = psumb()
            for c in range(nchunks):
                tr(pb[:, c * 128:(c + 1) * 128], hb[:, c * 128:(c + 1) * 128])
            cp("act", dstT[:, 0:nchunks, col0:col0 + 128],
               pb[:, 0:nchunks * 128].f(lambda a: a.rearrange("p (c t) -> p c t", t=128)))

        def out_proj_phase(wout_dram, sq):
            if 'P4' not in PH:
                return
            with ExitStack() as ph:
                wst = [sb(ph, "wo_st%d" % i, [64, 16, 256], F32) for i in range(2)]
                wo = sb(ph, "wo_b", [64, 16, D], BF16)
                og = [sb(ph, "og%d" % i, [64, 16, 512], BF16) for i in range(2)]
                xt = [sb(ph, "op_x%d" % i, [128, D], F32) for i in range(2)]
                wv = wout_dram.f(lambda a: a.rearrange("(h d) n -> d h n", d=64))
                for i in range(4):
                    ld(wst[i % 2], wv[:, :, i * 256:(i + 1) * 256])
                    cp("act" if i % 2 else "dve", wo[:, :, i * 256:(i + 1) * 256], wst[i % 2])
                ov = oT_d.f(lambda a: a.rearrange("h d t -> d h t"))
                for tg in range(4):
                    ld(og[tg % 2], ov[:, :, tg * 512:(tg + 1) * 512])
                    for t4 in range(4):
                        tok0 = sq * S + tg * 512 + t4 * 128
                        x_t = xt[t4 % 2]
                        ld(x_t, xres[tok0:tok0 + 128, :])
                        for half in range(2):
                            ps = psum()
                            for h in range(16):
                                mm(ps, og[tg % 2][:, h, t4 * 128:(t4 + 1) * 128], wo[:, h, half * 512:(half + 1) * 512],
                                   start=(h == 0), stop=(h == 15))
                            tt("dve", x_t[:, half * 512:(half + 1) * 512], ps, x_t[:, half * 512:(half + 1) * 512], ALU.add)
                        E.dma("pool", xres[tok0:tok0 + 128, :], x_t)
                E.barrier()

        def pv_norm_store(exps, vfn, M, ncols, dst_fn, tmp_r, tmp_o):
            po = psum()
            pz = psum()
            n = len(exps)
            for i, ex in enumerate(exps):
                mm(po[0:64, 0:ncols], vfn(i), ex, start=(i == 0), stop=(i == n - 1))
            for i, ex in enumerate(exps):
                mm(pz[0:64, 0:ncols], ones_b[:, 0:64], ex, start=(i == 0), stop=(i == n - 1))
            recip(tmp_r[0:64, 0:ncols], pz[0:64, 0:ncols])
            tt("dve", tmp_o[0:64, 0:ncols], po[0:64, 0:ncols], tmp_r[0:64, 0:ncols], ALU.mult)
            dst_fn(tmp_o)

        FM_COLS = [QA0 + 128 * j for j in range(4)] + [IQ0 + 128 * j for j in range(4)] + \
                  [QB0 + 128 * j for j in range(4)] + [KB0 + 128 * j for j in range(4)]
        CH_QA, CH_IQ, CH_QB, CH_KB, CH_KA, CH_IK = 0, 4, 8, 12, 16, 17

        def even_layer(li):
            j = li // 2
            w_in = hyb_in[j]
            with ExitStack() as L:
                sel = sb(L, "sel", [128, 64, 128], BF16)
                E.op("dve", lambda: nc.vector.tensor_copy(
                    out=sel.ap[0:64], in_=identb.ap[0:64, 0:64].unsqueeze(2).to_broadcast([64, 64, 128])), [identb], [sel])
                E.op("dve", lambda: nc.vector.tensor_copy(
                    out=sel.ap[64:128], in_=identb.ap[64:128, 64:128].unsqueeze(2).to_broadcast([64, 64, 128])), [identb], [sel])
                ld(gA, V(anorm.ap[li, :].partition_broadcast(128), anorm.res))
                wv = w_in.f(lambda a: a.rearrange("(c p) n -> p c n", p=128))

                def load_even_weights(stack):
                    Wfm = sb(stack, "Wfm", [128, 8, 18, 128], BF16)
                    Wsw = sb(stack, "Wsw", [128, 8, 18, 128], BF16)
                    Wtok = sb(stack, "Wtok", [128, 8, 584], BF16)
                    with ExitStack() as ph0:
                        stg = [sb(ph0, "wstg%d" % i, [128, 8, 128], F32) for i in range(2)]
                        stk = sb(ph0, "wstk", [128, 8, 584], F32)
                        for ci in range(18):
                            s_ = stg[ci % 2]
                            if ci < 16:
                                ld(s_, wv[:, :, FM_COLS[ci]:FM_COLS[ci] + 128])
                            else:
                                c0 = KA0 if ci == CH_KA else IK0
                                ld(s_[:, :, 0:64], wv[:, :, c0:c0 + 64])
                                ld(s_[:, :, 64:128], wv[:, :, c0:c0 + 64])
                            cp("act", Wfm[:, :, ci, :], s_)
                            sv = s_.f(lambda a: a.rearrange("p c (h t e) -> p c h t e", h=2, t=2))
                            ov = Wsw[:, :, ci, :].f(lambda a: a.rearrange("p c (h t e) -> p c h t e", h=2, t=2))
                            cp("dve", ov[:, :, :, 0, :], sv[:, :, :, 1, :])
                            cp("dve", ov[:, :, :, 1, :], sv[:, :, :, 0, :])
                        ld(stk[:, :, 0:64], wv[:, :, VA0:VA0 + 64])
                        ld(stk[:, :, 64:72], wv[:, :, IW0:IW0 + 8])
                        ld(stk[:, :, 72:584], wv[:, :, VB0:VB0 + 512])
                        cp("act", Wtok, stk)
                        E.barrier()
                    return Wfm, Wsw, Wtok

                for sq in range(spc):
                    with ExitStack() as SQ:
                        km = sb(SQ, "km", [128, 4, 8], F32)
                        kmb = sb(SQ, "kmb", [128, 4, 8], BF16)
                        with ExitStack() as ph:
                          if 'P1' in PH:
                            Wfm, Wsw, Wtok = load_even_weights(ph)
                            Ct = sb(ph, "Ct", [128, S], F32)
                            St = sb(ph, "St", [128, S], F32)
                            ld(Ct, tabC64[sq])
                            ld(St, tabS64[sq])
                            xt = [sb(ph, "p1x%d" % i, [128, D], F32) for i in range(2)]
                            hb = [sb(ph, "p1h%d" % i, [128, D], BF16) for i in range(2)]
                            hT = sb(ph, "hT", [128, 8, 512], BF16)
                            t1 = [sb(ph, "p1t1_%d" % i, [128, 512], F32) for i in range(2)]
                            t2 = [sb(ph, "p1t2_%d" % i, [128, 512], F32) for i in range(2)]
                            o32 = [sb(ph, "p1o32_%d" % i, [128, 512], F32) for i in range(2)]
                            ob = [sb(ph, "p1ob_%d" % i, [128, 512], BF16) for i in range(2)]
                            vat = [sb(ph, "p1va%d" % i, [128, 64], BF16) for i in range(2)]
                            iwt = [sb(ph, "p1iw%d" % i, [128, 8], F32) for i in range(2)]
                            vbt = [sb(ph, "p1vb%d" % i, [128, 512], BF16) for i in range(2)]
                            for tg in range(4):
                                for t4 in range(4):
                                    tok0 = sq * S + tg * 512 + t4 * 128
                                    ld(xt[t4 % 2], xres[tok0:tok0 + 128, :])
                                    norm_tile(xt[t4 % 2], gA, hb[t4 % 2], st1)
                                    transpose_to(hb[t4 % 2], hT, t4 * 128)
                                cs = slice(tg * 512, (tg + 1) * 512)
                                for ci in range(18):
                                    A = psum()
                                    B = psum()
                                    for c in range(8):
                                        mm(A, Wfm[:, c, ci, :], hT[:, c, :], start=(c == 0), stop=(c == 7))
                                    for c in range(8):
                                        mm(B, Wsw[:, c, ci, :], hT[:, c, :], start=(c == 0), stop=(c == 7))
                                    i2 = ci % 2
                                    tt("dve", t1[i2], A, Ct[:, cs], ALU.mult)
                                    tt("dve", t2[i2], B, St[:, cs], ALU.mult)
                                    if CH_KB <= ci < CH_KB + 4:
                                        tt("dve", o32[i2], t1[i2], t2[i2], ALU.add)
                                        E.op("dve", lambda: nc.vector.tensor_reduce(
                                            out=km.ap[:, ci - CH_KB, 2 * tg:2 * tg + 2],
                                            in_=o32[i2].ap.rearrange("p (b k) -> p b k", k=256), axis=AX.X, op=ALU.add),
                                            [o32[i2]], [km])
                                        cp("act", ob[i2], o32[i2])
                                    else:
                                        tt("dve", ob[i2], t1[i2], t2[i2], ALU.add)
                                    E.dma("pool", fmT[ci][:, cs], ob[i2])
                                for t4 in range(4):
                                    r0 = tg * 512 + t4 * 128
                                    p1_ = psum()
                                    p2_ = psum()
                                    for c in range(8):
                                        mm(p1_[:, 0:72], hT[:, c, t4 * 128:(t4 + 1) * 128], Wtok[:, c, 0:72], start=(c == 0), stop=(c == 7))
                                    for c in range(8):
                                        mm(p2_, hT[:, c, t4 * 128:(t4 + 1) * 128], Wtok[:, c, 72:584], start=(c == 0), stop=(c == 7))
                                    i2 = t4 % 2
                                    cp("act", vat[i2], p1_[:, 0:64])
                                    cp("act", iwt[i2], p1_[:, 64:72])
                                    cp("act", vbt[i2], p2_)
                                    E.dma("pool", va_d[r0:r0 + 128, :], vat[i2])
                                    E.dma("pool", iw_d[r0:r0 + 128, :], iwt[i2])
                                    E.dma("pool", vb_d[r0:r0 + 128, :], vbt[i2])
                            ts("dve", kmb, km, 1.0 / 256.0, None, ALU.mult)
                            E.barrier()

                        with ExitStack() as ph:
                          if 'P2' in PH:
                            iq_s = sb(ph, "iq_s", [128, 4, S], BF16)
                            ik_s = sb(ph, "ik_s", [128, S], BF16)
                            qa_s = sb(ph, "qa_s", [128, 4, S], BF16)
                            ka_s = sb(ph, "ka_s", [128, S], BF16)
                            va_s = sb(ph, "va_s", [128, NT, 64], BF16)
                            iw_s = sb(ph, "iw_s", [128, NT, 8], F32)
                            score = sb(ph, "score", [128, S], F32)
                            work = sb(ph, "work", [128, S], F32)
                            rl = [sb(ph, "rl%d" % i, [128, 512], F32) for i in range(2)]
                            biasA = [sb(ph, "biasA%d" % i, [128, S], BF16) for i in range(2)]
                            m8 = sb(ph, "m8", [128, 8], F32)
                            exA = [sb(ph, "exA%d" % i, [128, NT, 512], BF16) for i in range(2)]
                            tr_ = sb(ph, "dsa_r", [64, 512], F32)
                            to_ = [sb(ph, "dsa_o%d" % i, [64, 512], BF16) for i in range(2)]
                            for c in range(4):
                                ld(iq_s[:, c, :], fmT[CH_IQ + c])
                                ld(qa_s[:, c, :], fmT[CH_QA + c])
                            ld(ik_s, fmT[CH_IK])
                            ld(ka_s, fmT[CH_KA])
                            ld(va_s, va_d.f(lambda a: a.rearrange("(t p) d -> p t d", p=128)))
                            ld(iw_s, iw_d.f(lambda a: a.rearrange("(t p) d -> p t d", p=128)))
                            nrl = [0]
                            score2 = [score, sb(ph, "score_b", [128, S], F32)]
                            tmpb = [sb(ph, "ixt%d" % i, [128, 512], F32) for i in range(2)]

                            def indexer(qt):
                                sc_ = score2[qt % 2]
                                nk = (qt + 1) * 128
                                qs = slice(qt * 128, (qt + 1) * 128)
                                for h in range(8):
                                    jj, base = h // 2, 64 * (h % 2)
                                    for kc in range((nk + 511) // 512):
                                        ncol = min(512, nk - kc * 512)
                                        ks = slice(kc * 512, kc * 512 + ncol)
                                        ps = psum()
                                        mm(ps[:, 0:ncol], iq_s[base:base + 64, jj, qs], ik_s[base:base + 64, ks])
                                        r_ = rl[nrl[0] % 2]
                                        t_ = tmpb[nrl[0] % 2]
                                        nrl[0] += 1
                                        act(r_[:, 0:ncol], ps[:, 0:ncol], AF.Relu)
                                        if h == 0:
                                            E.op("act", lambda: nc.scalar.mul(out=sc_.ap[:, ks], in_=r_.ap[:, 0:ncol],
                                                                               mul=iw_s.ap[:, qt, 0:1]), [r_, iw_s], [sc_])
                                        else:
                                            E.op("act", lambda: nc.scalar.mul(out=t_.ap[:, 0:ncol], in_=r_.ap[:, 0:ncol],
                                                                               mul=iw_s.ap[:, qt, h:h + 1]), [r_, iw_s], [t_])
                                            tt("pool", sc_[:, ks], sc_[:, ks], t_[:, 0:ncol], ALU.add)
                                tt("pool", sc_[:, qs], sc_[:, qs], cst[:, C_TRIQK:C_TRIQK + 128], ALU.add)

                            indexer(0)
                            for qt in range(NT):
                                if qt + 1 < NT:
                                    indexer(qt + 1)
                                score = score2[qt % 2]
                                nk = (qt + 1) * 128
                                qs = slice(qt * 128, (qt + 1) * 128)
                                bA = biasA[qt % 2]
                                if qt >= 2:
                                    cur = score
                                    for r in range(32):
                                        E.op("dve", lambda: nc.vector.max(out=m8.ap, in_=cur.ap[:, 0:nk]), [cur], [m8])
                                        if r < 31:
                                            E.op("dve", lambda: nc.vector.match_replace(
                                                out=work.ap[:, 0:nk], in_to_replace=m8.ap, in_values=cur.ap[:, 0:nk],
                                                imm_value=NEGBIG), [m8, cur], [work])
                                            cur = work
                                    thr = m8[:, 7:8]
                                else:
                                    thr = cst[:, C_M1E29:C_M1E29 + 1]
                                ts("dve", bA[:, 0:nk], score[:, 0:nk], thr, NEG, ALU.is_lt, ALU.mult)
                                for hg in range(2):
                                    base = 64 * hg
                                    ex = exA[hg]
                                    for kt in range(qt + 1):
                                        ps = psum()
                                        ps3 = ps.f(lambda a: a.rearrange("p (a b) -> p a b", b=128))
                                        mm(ps3, ka_s[base:base + 64, kt * 128:(kt + 1) * 128], qa_s[base:base + 64, :, qs],
                                           start=True, stop=False)
                                        mm(ps3, bA[:, kt * 128:(kt + 1) * 128], i4b, start=False, stop=True)
                                        act(ex[:, kt, :], ps, AF.Exp, scale=0.125)

                                    def dst(tmp_o, hg=hg, qs=qs):
                                        dv = V(oT_d.ap[0:8].rearrange("(j two) d t -> two j d t", two=2)[hg][:, :, qs].rearrange("j d t -> d j t"), oT_d.res)
                                        E.dma("sp", dv, tmp_o[:, :].f(lambda a: a.rearrange("p (h t) -> p h t", t=128)))
                                    pv_norm_store([ex[:, kt, :] for kt in range(qt + 1)],
                                                  lambda i: va_s[:, i, :], 64, 512, dst, tr_, to_[hg])
                            E.barrier()

                        with ExitStack() as ph:
                          if 'P3' in PH:
                            qb_s = sb(ph, "qb_s", [128, 4, S], BF16)
                            kb_s = sb(ph, "kb_s", [128, 4, S], BF16)
                            vb_s = sb(ph, "vb_s", [128, NT, 512], BF16)
                            bmT = sb(ph, "bmT", [128, S], BF16)
                            gm = sb(ph, "gm", [128, 8, 16], F32)
                            m8b = sb(ph, "m8b", [128, 8, 8], F32)
                            bq = sb(ph, "bq", [128, 8, 8], F32)
                            bqb = sb(ph, "bqb", [128, 128], BF16)
                            memset("dve", bqb, 0.0)
                            exB = [sb(ph, "exB%d" % i, [128, NT, 256], BF16) for i in range(2)]
                            tr_ = sb(ph, "mb_r", [64, 512], F32)
                            to_ = [sb(ph, "mb_o%d" % i, [64, 512], BF16) for i in range(2)]
                            for c in range(4):
                                ld(qb_s[:, c, :], fmT[CH_QB + c])
                                ld(kb_s[:, c, :], fmT[CH_KB + c])
                            ld(vb_s, vb_d.f(lambda a: a.rearrange("(t p) d -> p t d", p=128)))
                            kmpad = sb(ph, "kmpad", [128, 4, 128], BF16)
                            memset("dve", kmpad, 0.0)
                            cp("dve", kmpad[:, :, 0:8], kmb)
                            bqa = sb(ph, "bqa", [128, NT, 128], BF16)
                            memset("dve", bqa, 0.0)
                            if P3VAR == 'zerob':
                                memset("dve", bmT, 0.0)
                            for qt in (range(NT) if P3VAR != 'zerob' else []):
                                own = qt // 2
                                qs = slice(qt * 128, (qt + 1) * 128)
                                if own <= 3:
                                    continue
                                memset("dve", gm, NEGBIG)
                                gmv = gm.f(lambda a: a.rearrange("p (j two) n -> p two j n", two=2))
                                for par in range(2):
                                    ps = psum()
                                    base = 64 * par
                                    for jj in range(4):
                                        mm(ps[:, jj * 128:(jj + 1) * 128], qb_s[base:base + 64, jj, qs], kmpad[base:base + 64, jj, :])
                                    cp("dve", gmv[:, par, :, 0:own],
                                       ps.f(lambda a: a.rearrange("p (h n) -> p h n", n=128))[:, :, 0:own])
                                for h in range(8):
                                    E.op("dve", lambda: nc.vector.max(out=m8b.ap[:, h, :], in_=gm.ap[:, h, :]), [gm], [m8b])
                                E.op("dve", lambda: nc.vector.tensor_tensor(
                                    out=bq.ap, in0=gm.ap[:, :, 0:8], in1=m8b.ap[:, :, 2:3].to_broadcast([128, 8, 8]), op=ALU.is_lt),
                                    [gm, m8b], [bq])
                                ts("dve", bqa[:, qt, 0:64], bq.f(lambda a: a.rearrange("p h n -> p (h n)")), NEG, None, ALU.mult)
                                ts("dve", bqa[:, qt, 64:128], bq.f(lambda a: a.rearrange("p h n -> p (h n)")), NEG, None, ALU.mult)
                            for qt in (range(NT) if P3VAR != 'zerob' else []):
                                qs = slice(qt * 128, (qt + 1) * 128)
                                pt_ = psum()
                                mm(pt_[:, 0:128], bqa[:, qt, :], identb)
                                cp("dve", bmT[:, qs], pt_[:, 0:128])
                            it = 0
                            for h in (range(8) if 'P3b' in PH else []):
                                jj, base = h // 2, 64 * (h % 2)
                                for nb in range(8):
                                    ex = exB[it % 2]
                                    q2 = slice(nb * 256, (nb + 1) * 256)
                                    nkt = 2 * nb + 2
                                    for kt in range(nkt):
                                        n = kt // 2
                                        ksl = slice(kt * 128, (kt + 1) * 128)
                                        ps = psum()
                                        if n < nb:
                                            mm(ps[:, 0:256], kb_s[base:base + 64, jj, ksl], qb_s[base:base + 64, jj, q2], start=True, stop=False)
                                            mm(ps[:, 0:256], sel[base:base + 64, h * 8 + n, :], bmT[base:base + 64, q2], start=False, stop=True)
                                            act(ex[:, kt, :], ps[:, 0:256], AF.Exp, scale=0.125)
                                        elif kt == 2 * nb:
                                            qA = slice(nb * 256, nb * 256 + 128)
                                            qB = slice(nb * 256 + 128, (nb + 1) * 256)
                                            mm(ps[:, 0:128], kb_s[base:base + 64, jj, ksl], qb_s[base:base + 64, jj, qA], start=True, stop=False)
                                            mm(ps[:, 0:128], identb, tritb, start=False, stop=True)
                                            mm(ps[:, 128:256], kb_s[base:base + 64, jj, ksl], qb_s[base:base + 64, jj, qB], start=True, stop=True)
                                            act(ex[:, kt, :], ps[:, 0:256], AF.Exp, scale=0.125)
                                        else:
                                            q1 = slice(nb * 256 + 128, (nb + 1) * 256)
                                            mm(ps[:, 0:128], kb_s[base:base + 64, jj, ksl], qb_s[base:base + 64, jj, q1], start=True, stop=False)
                                            mm(ps[:, 0:128], identb, tritb, start=False, stop=True)
                                            memset("pool", ex[:, kt, 0:128], 0.0)
                                            act(ex[:, kt, 128:256], ps[:, 0:128], AF.Exp, scale=0.125)

                                    def dst(tmp_o, h=h, q2=q2):
                                        E.dma("pool", oT_d[8 + h][:, q2], tmp_o[:, 0:256])
                                    pv_norm_store([ex[:, kt, :] for kt in range(nkt)],
                                                  lambda i, h=h: vb_s[:, i, h * 64:(h + 1) * 64], 64, 256, dst, tr_, to_[it % 2])
                                    it += 1
                            E.barrier()
                    out_proj_phase(hyb_out[j], sq)

        def odd_layer(li):
            j = li // 2
            with ExitStack() as L:
                Win = sb(L, "m_Win", [128, 8, 1024], BF16)
                Wkr = sb(L, "m_Wkr", [128, 8, 128], BF16)
                Wkrs = sb(L, "m_Wkrs", [128, 8, 128], BF16)
                Wuq = sb(L, "m_Wuq", [128, 6, 16, 128], BF16)
                Wuqs = sb(L, "m_Wuqs", [128, 6, 16, 128], BF16)
                Wk = sb(L, "m_Wk", [128, 2, 16, 128], BF16)
                Wv = sb(L, "m_Wv", [128, 2, 16, 64], BF16)
                gq = sb(L, "m_gq", [128, 8], F32)
                ld(gA, V(anorm.ap[li, :].partition_broadcast(128), anorm.res))
                ld(gq, mla_gq[j])
                with ExitStack() as ph:
                    stg = [sb(ph, "mstg%d" % i, [128, 8, 256], F32) for i in range(2)]
                    wv = mla_in[j].f(lambda a: a.rearrange("(c p) n -> p c n", p=128))
                    for i in range(4):
                        ld(stg[i % 2], wv[:, :, i * 256:(i + 1) * 256])
                        cp("act" if i % 2 else "dve", Win[:, :, i * 256:(i + 1) * 256], stg[i % 2])
                    memset("dve", Wkr, 0.0)
                    memset("dve", Wkrs, 0.0)
                    s_ = stg[0]
                    ld(s_[:, :, 0:32], wv[:, :, 1024:1056])
                    cp("dve", Wkr[:, :, 64:96], s_[:, :, 0:32])
                    cp("dve", Wkrs[:, :, 64:80], s_[:, :, 16:32])
                    cp("dve", Wkrs[:, :, 80:96], s_[:, :, 0:16])
                    uv = mla_uq[j].f(lambda a: a.rearrange("(c p) n -> p c n", p=128))
                    memset("dve", Wuq, 0.0)
                    memset("dve", Wuqs, 0.0)
                    stq = sb(ph, "mstq", [128, 6, 768], F32)
                    for i in range(2):
                        ld(stq, uv[:, :, i * 768:(i + 1) * 768])
                        s4 = stq.f(lambda a: a.rearrange("p c (h e) -> p c h e", e=96))
                        cp("act", Wuq[:, :, 8 * i:8 * i + 8, 0:96], s4)
                        cp("dve", Wuqs[:, :, 8 * i:8 * i + 8, 0:64], s4[:, :, :, 0:64])
                        cp("dve", Wuqs[:, :, 8 * i:8 * i + 8, 64:80], s4[:, :, :, 80:96])
                        cp("dve", Wuqs[:, :, 8 * i:8 * i + 8, 80:96], s4[:, :, :, 64:80])
                    kvv = mla_ukv[j].f(lambda a: a.rearrange("(c p) n -> p c n", p=128))
                    memset("dve", Wk, 0.0)
                    for i in range(8):
                        s_ = stg[i % 2][:, 0:2, :]
                        ld(s_, kvv[:, :, i * 256:(i + 1) * 256])
                        s4 = s_.f(lambda a: a.rearrange("p c (h e) -> p c h e", e=128))
                        cp("dve", Wk[:, :, 2 * i:2 * i + 2, 0:64], s4[:, :, :, 0:64])
                        cp("act", Wv[:, :, 2 * i:2 * i + 2, :], s4[:, :, :, 64:128])
                    E.barrier()

                for sq in range(spc):
                    with ExitStack() as ph:
                      if 'P1' in PH:
                        Ct = sb(ph, "mCt", [128, S], F32)
                        St = sb(ph, "mSt", [128, S], F32)
                        ld(Ct, tabC96[sq])
                        ld(St, tabS96[sq])
                        xt = [sb(ph, "m1x%d" % i, [128, D], F32) for i in range(2)]
                        hb = [sb(ph, "m1h%d" % i, [128, D], BF16) for i in range(2)]
                        hT = sb(ph, "mhT", [128, 8, 512], BF16)
                        cT = sb(ph, "mcT", [128, 8, 512], F32)
                        sqb = sb(ph, "msq", [128, 8, 512], BF16)
                        cn = sb(ph, "mcn", [128, 8, 512], BF16)
                        rq = sb(ph, "mrq", [128, 2, 512], F32)
                        kr96 = sb(ph, "mkr96", [128, 512], F32)
                        t1 = [sb(ph, "m1t1_%d" % i, [128, 512], F32) for i in range(2)]
                        t2 = [sb(ph, "m1t2_%d" % i, [128, 512], F32) for i in range(2)]
                        ob = [sb(ph, "m1ob_%d" % i, [128, 512], BF16) for i in range(2)]
                        kb_ = [sb(ph, "m1kb_%d" % i, [128, 512], BF16) for i in range(2)]
                        vt = [sb(ph, "m1vt%d" % i, [128, 1024], BF16) for i in range(2)]
                        for tg in range(4):
                            if MSTEP < 2:
                                continue
                            cs = slice(tg * 512, (tg + 1) * 512)
                            for t4 in range(4):
                                tok0 = sq * S + tg * 512 + t4 * 128
                                ld(xt[t4 % 2], xres[tok0:tok0 + 128, :])
                                norm_tile(xt[t4 % 2], gA, hb[t4 % 2], st1)
                                transpose_to(hb[t4 % 2], hT, t4 * 128)
                            for c in range(8):
                                ps = psum()
                                for d_ in range(8):
                                    mm(ps, Win[:, d_, c * 128:(c + 1) * 128], hT[:, d_, :], start=(d_ == 0), stop=(d_ == 7))
                                cp("dve", cT[:, c, :], ps)
                                act(sqb[:, c, :], ps, AF.Square)
                            if MSTEP < 3:
                                continue
                            for (gi, c0, c1, n_) in ((0, 0, 6, 768), (1, 6, 8, 256)):
                                ps = psum()
                                for c in range(c0, c1):
                                    mm(ps, ones_b, sqb[:, c, :], start=(c == c0), stop=(c == c1 - 1))
                                act(rq[:, gi, :], ps, AF.Sqrt, scale=1.0 / n_, bias=EPS)
                                recip(rq[:, gi, :], rq[:, gi, :])
                                for c in range(c0, c1):
                                    stt(cn[:, c, :], cT[:, c, :], gq[:, c:c + 1], rq[:, gi, :], ALU.mult, ALU.mult)
                            if MSTEP < 4:
                                continue
                            A = psum()
                            B = psum()
                            for d_ in range(8):
                                mm(A, Wkr[:, d_, :], hT[:, d_, :], start=(d_ == 0), stop=(d_ == 7))
                            for d_ in range(8):
                                mm(B, Wkrs[:, d_, :], hT[:, d_, :], start=(d_ == 0), stop=(d_ == 7))
                            tt("dve", t1[0], A, Ct[:, cs], ALU.mult)
                            tt("dve", t2[0], B, St[:, cs], ALU.mult)
                            tt("dve", kr96, t1[0], t2[0], ALU.add)
                            if MSTEP < 5:
                                continue
                            for h in range(16):
                                i2 = h % 2
                                A = psum()
                                B = psum()
                                for c in range(6):
                                    mm(A, Wuq[:, c, h, :], cn[:, c, :], start=(c == 0), stop=(c == 5))
                                for c in range(6):
                                    mm(B, Wuqs[:, c, h, :], cn[:, c, :], start=(c == 0), stop=(c == 5))
                                tt("dve", t1[i2], A, Ct[:, cs], ALU.mult)
                                tt("dve", t2[i2], B, St[:, cs], ALU.mult)
                                tt("dve", ob[i2], t1[i2], t2[i2], ALU.add)
                                E.dma("pool", qT_d[h][:, cs], ob[i2])
                                K = psum()
                                for c in range(2):
                                    mm(K, Wk[:, c, h, :], cn[:, 6 + c, :], start=(c == 0), stop=(c == 1))
                                tt("dve", kb_[i2], K, kr96, ALU.add)
                                E.dma("pool", kT_d[h][:, cs], kb_[i2])
                            if MSTEP < 6:
                                continue
                            for t4 in range(4):
                                r0 = tg * 512 + t4 * 128
                                i2 = t4 % 2
                                for half in range(2):
                                    ps = psum()
                                    for c in range(2):
                                        mm(ps, cn[:, 6 + c, t4 * 128:(t4 + 1) * 128],
                                           Wv[:, c, half * 8:(half + 1) * 8, :].f(lambda a: a.rearrange("p h e -> p (h e)")),
                                           start=(c == 0), stop=(c == 1))
                                    cp("act", vt[i2][:, half * 512:(half + 1) * 512], ps)
                                E.dma("pool", V_d[r0:r0 + 128, :], vt[i2])
                        E.barrier()
                    with ExitStack() as ph:
                      if 'P2' in PH:
                        V_s = sb(ph, "mV_s", [128, NT, 1024], BF16)
                        qh = [sb(ph, "mqh%d" % i, [128, S], BF16) for i in range(2)]
                        kh = [sb(ph, "mkh%d" % i, [128, S], BF16) for i in range(2)]
                        exM = [sb(ph, "exM%d" % i, [128, NT, 512], BF16) for i in range(2)]
                        tr_ = sb(ph, "ml_r", [64, 512], F32)
                        to_ = [sb(ph, "ml_o%d" % i, [64, 512], BF16) for i in range(2)]
                        ld(V_s, V_d.f(lambda a: a.rearrange("(t p) d -> p t d", p=128)))
                        sc_m = 1.0 / math.sqrt(96.0)
                        it = 0
                        for h in range(16):
                            q_h = qh[h % 2]
                            k_h = kh[h % 2]
                            ld(q_h, qT_d[h])
                            ld(k_h, kT_d[h])
                            for qg in range(4):
                                ex = exM[it % 2]
                                nkt = 4 * qg + 4
                                for kt in range(nkt):
                                    lo = max(kt, 4 * qg)
                                    ncol = (4 * qg + 4 - lo) * 128
                                    off = (lo - 4 * qg) * 128
                                    ps = psum()
                                    diag = kt >= 4 * qg
                                    if not diag:
                                        mm(ps[:, 0:ncol], k_h[:, kt * 128:(kt + 1) * 128], q_h[:, lo * 128:(4 * qg + 4) * 128],
                                           start=True, stop=True)
                                    else:
                                        mm(ps[:, 0:128], k_h[:, kt * 128:(kt + 1) * 128], q_h[:, lo * 128:(lo + 1) * 128],
                                           start=True, stop=False)
                                        mm(ps[:, 0:128], identb, tritb, start=False, stop=True)
                                        if ncol > 128:
                                            mm(ps[:, 128:ncol], k_h[:, kt * 128:(kt + 1) * 128],
                                               q_h[:, (lo + 1) * 128:(4 * qg + 4) * 128], start=True, stop=True)
                                        if off > 0:
                                            memset("pool", ex[:, kt, 0:off], 0.0)
                                    act(ex[:, kt, off:512], ps[:, 0:ncol], AF.Exp, scale=sc_m)

                                def dst(tmp_o, h=h, qg=qg):
                                    E.dma("pool", oT_d[h][:, qg * 512:(qg + 1) * 512], tmp_o)
                                pv_norm_store([ex[:, kt, :] for kt in range(nkt)],
                                              lambda i, h=h: V_s[:, i, h * 64:(h + 1) * 64], 64, 512, dst, tr_, to_[it % 2])
                                it += 1
                        E.barrier()
                    out_proj_phase(mla_out[j], sq)

        def peer_layer(li):
            if 'PEER' not in PH:
                return
            with ExitStack() as L:
                Wq = sb(L, "p_Wq", [128, 8, 2048], BF16)
                skT = sb(L, "p_skT", [128, 16, 128], BF16)
                gF = sb(L, "p_gF", [128, D], F32)
                ld(gF, V(fnorm.ap[li, :].partition_broadcast(128), fnorm.res))
                with ExitStack() as ph:
                    stg = [sb(ph, "pstg%d" % i, [128, 8, 256], F32) for i in range(2)]
                    skf = [sb(ph, "pskf%d" % i, [128, 128], F32) for i in range(2)]
                    skb = [sb(ph, "pskb%d" % i, [128, 128], BF16) for i in range(2)]
                    wv = peer_wq[li].f(lambda a: a.rearrange("(c p) n -> p c n", p=128))
                    for i in range(8):
                        ld(stg[i % 2], wv[:, :, i * 256:(i + 1) * 256])
                        cp("act" if i % 2 else "dve", Wq[:, :, i * 256:(i + 1) * 256], stg[i % 2])
                    cin = [sb(ph, "pcin%d" % i, [128, 2, 2048], F32) for i in range(2)]
                    cout = [sb(ph, "pcout%d" % i, [128, 2, 2048], BF16) for i in range(3)]
                    tv = peer_uv[li].f(lambda a: a.rearrange("(p r) c -> p r c", p=128))
                    ov_ = uvb[li].f(lambda a: a.rearrange("(p r) c -> p r c", p=128))
                    engs = ("act", "dve", "pool")
                    for ch in range(64):
                        ld(cin[ch % 2], tv[:, 2 * ch:2 * ch + 2, :], q=("sp", "act")[ch % 2])
                        cp(engs[ch % 3], cout[ch % 3], cin[ch % 2])
                        E.dma("pool", ov_[:, 2 * ch:2 * ch + 2, :], cout[ch % 3])
                    for hp in range(16):
                        ld(skf[hp % 2], peer_sk[li][hp])
                        cp("dve", skb[hp % 2], skf[hp % 2])
                        pb = psumb()
                        tr(pb[:, 0:128], skb[hp % 2])
                        cp("act", skT[:, hp, :], pb[:, 0:128])
                    E.barrier()
                with ExitStack() as ph:
                    xt = [sb(ph, "px%d" % i, [128, D], F32) for i in range(2)]
                    h32 = sb(ph, "ph32", [128, D], F32)
                    hb = sb(ph, "phb", [128, D], BF16)
                    hT = sb(ph, "phT", [128, 8, 128], BF16)
                    qT = sb(ph, "pqT", [128, 16, 128], BF16)
                    bufA = sb(ph, "pbufA", [128, 2048], F32)
                    bufB = sb(ph, "pbufB", [128, 2048], F32)
                    sc = bufA.f(lambda a: a.rearrange("p (a b) -> p a b", b=128))
                    scw = bufB.f(lambda a: a.rearrange("p (a b) -> p a b", b=128))
                    vh = sb(ph, "pvh", [128, 16, 16], F32)
                    ih = sb(ph, "pih", [128, 16, 16], U32)
                    ihf = sb(ph, "pihf", [128, 16, 16], F32)
                    cand = bufA.f(lambda a: a.rearrange("p (a b) -> p a b", b=256))
                    candw = bufB.f(lambda a: a.rearrange("p (a b) -> p a b", b=256))
                    cidx = sb(ph, "pcidx", [128, 8, 256], F32)
                    tops = sb(ph, "ptops", [128, 8, 16], F32)
                    posu = sb(ph, "pposu", [128, 8, 16], U32)
                    posf = sb(ph, "pposf", [128, 8, 16], F32)
                    expf = sb(ph, "pexpf", [128, 128], F32)
                    expu = sb(ph, "pexpu", [128, 128], I32)
                    ohj = sb(ph, "pohj", [128, 256], F32)
                    gsm = sb(ph, "pgsm", [128, 8, 16], F32)
                    gsum = sb(ph, "pgsum", [128, 8], F32)
                    actv = sb(ph, "pactv", [128, 128], F32)
                    wgt = sb(ph, "pwgt", [128, 128], F32)
                    NSL = 4
                    NSET = int(os.environ.get('NSET', '4'))
                    gb = [sb(ph, "pgb%d" % i, [128, NSL, 2048], BF16) for i in range(NSET)]
                    dj = sb(ph, "pdj", [128, D], F32)
                    NDG = 4
                    dg = [sb(ph, "pdg%d" % i, [128, 128], BF16) for i in range(NDG)]
                    rot[0] = 4
                    accA, accB = banks[4], banks[5]
                    iota = cst[:, C_IOTA:C_IOTA + 256]
                    gi = [0]
                    h32s = [h32, sb(ph, "ph32b", [128, D], F32)]
                    expus = [expu, sb(ph, "pexpub", [128, 128], I32)]
                    gsms = [gsm, sb(ph, "pgsmb", [128, 8, 16], F32)]

                    def stageA(t):
                        x_t = xt[t % 2]
                        h32 = h32s[t % 2]
                        expu = expus[t % 2]
                        gsm = gsms[t % 2]
                        ld(x_t, xres[t * 128:(t + 1) * 128, :])
                        norm_tile(x_t, gF, h32, st1)
                        cp("act", hb, h32)
                        transpose_to(hb, hT, 0)
                        for g4 in range(4):
                            ps = psum()
                            for k4 in range(4):
                                hp = g4 * 4 + k4
                                for c in range(8):
                                    mm(ps[:, k4 * 128:(k4 + 1) * 128], Wq[:, c, hp * 128:(hp + 1) * 128], hT[:, c, :],
                                       start=(c == 0), stop=(c == 7))
                            cp("act", qT[:, g4 * 4:(g4 + 1) * 4, :], ps.f(lambda a: a.rearrange("p (k t) -> p k t", t=128)))
                        for g4 in range(4):
                            ps = psum()
                            for k4 in range(4):
                                hp = g4 * 4 + k4
                                mm(ps[:, k4 * 128:(k4 + 1) * 128], qT[:, hp, :], skT[:, hp, :])
                            cp("act", sc[:, g4 * 4:(g4 + 1) * 4, :], ps.f(lambda a: a.rearrange("p (k t) -> p k t", t=128)))
                        yield
                        for hp in range(16):
                            E.op("dve", lambda: nc.vector.max(out=vh.ap[:, hp, 0:8], in_=sc.ap[:, hp, :]), [sc], [vh])
                            E.op("dve", lambda: nc.vector.max_index(out=ih.ap[:, hp, 0:8], in_max=vh.ap[:, hp, 0:8],
                                                                    in_values=sc.ap[:, hp, :]), [vh, sc], [ih])
                            E.op("dve", lambda: nc.vector.match_replace(out=scw.ap[:, hp, :], in_to_replace=vh.ap[:, hp, 0:8],
                                                                        in_values=sc.ap[:, hp, :], imm_value=NEGBIG), [vh, sc], [scw])
                        yield
                        for hp in range(16):
                            E.op("dve", lambda: nc.vector.max(out=vh.ap[:, hp, 8:16], in_=scw.ap[:, hp, :]), [scw], [vh])
                            E.op("dve", lambda: nc.vector.max_index(out=ih.ap[:, hp, 8:16], in_max=vh.ap[:, hp, 8:16],
                                                                    in_values=scw.ap[:, hp, :]), [vh, scw], [ih])
                        yield
                        cp("dve", ihf, ih)
                        v4 = vh.f(lambda a: a.rearrange("p (h t) k -> p h t k", t=2))
                        i4 = ihf.f(lambda a: a.rearrange("p (h t) k -> p h t k", t=2))
                        for h in range(8):
                            E.op("dve", lambda: nc.vector.tensor_tensor(
                                out=cand.ap[:, h, :].rearrange("p (a b) -> p a b", b=16),
                                in0=v4.ap[:, h, 0, :].unsqueeze(2).to_broadcast([128, 16, 16]),
                                in1=v4.ap[:, h, 1, :].unsqueeze(1).to_broadcast([128, 16, 16]), op=ALU.add), [vh], [cand])
                            E.op("dve", lambda: nc.vector.scalar_tensor_tensor(
                                out=cidx.ap[:, h, :].rearrange("p (a b) -> p a b", b=16),
                                in0=i4.ap[:, h, 0, :].unsqueeze(2).to_broadcast([128, 16, 16]), scalar=128.0,
                                in1=i4.ap[:, h, 1, :].unsqueeze(1).to_broadcast([128, 16, 16]),
                                op0=ALU.mult, op1=ALU.add), [ihf], [cidx])
                        yield
                        for h in range(8):
                            E.op("dve", lambda: nc.vector.max(out=tops.ap[:, h, 0:8], in_=cand.ap[:, h, :]), [cand], [tops])
                            E.op("dve", lambda: nc.vector.max_index(out=posu.ap[:, h, 0:8], in_max=tops.ap[:, h, 0:8],
                                                                    in_values=cand.ap[:, h, :]), [tops, cand], [posu])
                            E.op("dve", lambda: nc.vector.match_replace(out=candw.ap[:, h, :], in_to_replace=tops.ap[:, h, 0:8],
                                                                        in_values=cand.ap[:, h, :], imm_value=NEGBIG), [tops, cand], [candw])
                        yield
                        for h in range(8):
                            E.op("dve", lambda: nc.vector.max(out=tops.ap[:, h, 8:16], in_=candw.ap[:, h, :]), [candw], [tops])
                            E.op("dve", lambda: nc.vector.max_index(out=posu.ap[:, h, 8:16], in_max=tops.ap[:, h, 8:16],
                                                                    in_values=candw.ap[:, h, :]), [tops, candw], [posu])
                        yield
                        cp("dve", posf, posu)
                        memset("dve", expf, 0.0)
                        for h in range(8):
                            yield
                            for k in range(16):
                                stt(ohj, iota, posf[:, h, k:k + 1], cidx[:, h, :], ALU.is_equal, ALU.mult,
                                    accum=expf[:, h * 16 + k:h * 16 + k + 1])
                        yield
                        cp("dve", expu, expf)
                        E.op("dve", lambda: nc.vector.tensor_tensor(
                            out=gsm.ap, in0=tops.ap, in1=tops.ap[:, :, 0:1].to_broadcast([128, 8, 16]), op=ALU.subtract), [tops], [gsm])
                        act(gsm, gsm, AF.Exp)
                        E.op("dve", lambda: nc.vector.tensor_reduce(out=gsum.ap, in_=gsm.ap, axis=AX.X, op=ALU.add), [gsm], [gsum])
                        recip(gsum, gsum)
                        E.op("dve", lambda: nc.vector.tensor_tensor(
                            out=gsm.ap, in0=gsm.ap, in1=gsum.ap.unsqueeze(2).to_broadcast([128, 8, 16]), op=ALU.mult), [gsm, gsum], [gsm])

                    def stageB(t):
                        x_t = xt[t % 2]
                        h32 = h32s[t % 2]
                        expu = expus[t % 2]
                        gsm = gsms[t % 2]
                        gflat = gsm.f(lambda a: a.rearrange("p h k -> p (h k)"))
                        memset("dve", actv, 0.0)
                        first_acc = True
                        for g_ in range(128 // NSL):
                            if genA[0] is not None and g_ % 2 == 0:
                                next(genA[0], None)
                            buf = gb[gi[0] % NSET]
                            gi[0] += 1
                            for s_ in (range(NSL) if PEERVAR != 'nogather' else []):
                                sl = g_ * NSL + s_
                                E.dma("pool", buf[:, s_, :], uvb[li],
                                      fn=(lambda: nc.gpsimd.indirect_dma_start(
                                          out=buf.ap[:, s_, 0:1024], out_offset=None, in_=peer_uv[li].ap[:, 0:1024],
                                          in_offset=bass.IndirectOffsetOnAxis(ap=expu.ap[:, sl:sl + 1], axis=0))) if PEERVAR == 'half' else lambda: nc.gpsimd.indirect_dma_start(
                                          out=buf.ap[:, s_, :], out_offset=None, in_=uvb[li].ap,
                                          in_offset=bass.IndirectOffsetOnAxis(ap=expu.ap[:, sl:sl + 1], axis=0)),
                                      extra_reads=[expu])
                            if PEERVAR in ('nocompute', 'half'):
                                continue
                            for s_ in range(NSL):
                                sl = g_ * NSL + s_
                                stt(dj, buf[:, s_, 0:D], 1.0, h32, ALU.mult, ALU.mult, accum=actv[:, sl:sl + 1])
                            c0 = g_ * NSL
                            act(wgt[:, c0:c0 + NSL], actv[:, c0:c0 + NSL], AF.Gelu)
                            tt("dve", wgt[:, c0:c0 + NSL], wgt[:, c0:c0 + NSL], gflat[:, c0:c0 + NSL], ALU.mult)
                            for s_ in range(NSL):
                                sl = g_ * NSL + s_
                                d_ = dg[sl % NDG]
                                ts("dve", d_, identb, wgt[:, sl:sl + 1], None, ALU.mult)
                                mm(accA, d_, buf[:, s_, D:D + 512], start=(sl == 0), stop=(sl == 127))
                                mm(accB, d_, buf[:, s_, D + 512:2 * D], start=(sl == 0), stop=(sl == 127))
                        tt("dve", x_t[:, 0:512], accA, x_t[:, 0:512], ALU.add)
                        tt("dve", x_t[:, 512:1024], accB, x_t[:, 512:1024], ALU.add)
                        E.dma("sp", xres[t * 128:(t + 1) * 128, :], x_t)

                    ntile = ntok // 128
                    genA = [None]
                    for _ in stageA(0):
                        pass
                    for t in range(ntile):
                        genA[0] = stageA(t + 1) if t + 1 < ntile else None
                        stageB(t)
                        if genA[0] is not None:
                            for _ in genA[0]:
                                pass
                    E.barrier()
                    rot[0] = 6

        for li in layers:
            if li % 2 == 0:
                even_layer(li)
            else:
                odd_layer(li)
            peer_layer(li)

        if last:
            with ExitStack() as ph:
                xt = [sb(ph, "fx%d" % i, [128, D], F32) for i in range(2)]
                yo = [sb(ph, "fy%d" % i, [128, D], F32) for i in range(2)]
                ld(gA, V(finorm.ap[0, :].partition_broadcast(128), finorm.res))
                for t in range(ntok // 128):
                    ld(xt[t % 2], xres[t * 128:(t + 1) * 128, :])
                    norm_tile(xt[t % 2], gA, yo[t % 2], st1)
                    E.dma("sp", y_out[t * 128:(t + 1) * 128, :], yo[t % 2])
        E.barrier()
        E.finish()
        print("built program: %d instructions" % E.ninst, flush=True)
    return nc


def mla_gq_layout(qn, kvn):
    qn = np.asarray(qn, dtype=np.float32).reshape(2, 6, 128)
    kvn = np.asarray(kvn, dtype=np.float32).reshape(2, 2, 128)
    return np.ascontiguousarray(np.concatenate([qn, kvn], axis=1).transpose(0, 2, 1))


_CACHE = {}


def _get_prog(key, *a, **k):
    if key not in _CACHE:
        _CACHE[key] = build_program(*a, **k)
    return _CACHE[key]


def kernel(x, positions, attn_norm, ffn_norm, final_norm, hyb_w_in, hyb_w_out,
           mla_w_in, mla_q_norm, mla_kv_norm, mla_w_uq, mla_w_ukv, mla_w_out,
           peer_w_q, peer_sub_keys, peer_u, peer_v):
    f = lambda a: np.ascontiguousarray(np.asarray(a, dtype=np.float32))
    x = f(x).reshape(NCORES, SPC * S, D)
    pos = np.ascontiguousarray(np.asarray(positions, dtype=np.int32)).reshape(NCORES, SPC, S)
    peer_uv = np.concatenate([f(peer_u), f(peer_v)], axis=-1)
    common = {
        "cst": make_consts(),
        "attn_norm": f(attn_norm), "ffn_norm": f(ffn_norm), "final_norm": f(final_norm).reshape(1, D),
        "hyb_w_in": f(hyb_w_in), "hyb_w_out": f(hyb_w_out), "mla_w_in": f(mla_w_in),
        "mla_gq": mla_gq_layout(mla_q_norm, mla_kv_norm), "mla_w_uq": f(mla_w_uq),
        "mla_w_ukv": f(mla_w_ukv), "mla_w_out": f(mla_w_out), "peer_w_q": f(peer_w_q),
        "peer_sub_keys": f(peer_sub_keys).reshape(4, 16, 128, 128),
    }
    for i in range(4):
        common["peer_uv%d" % i] = peer_uv[i]
    nc = _get_prog("full", [0, 1, 2, 3], True, True)
    in_maps = []
    for c in range(NCORES):
        m = dict(common)
        m["x"] = x[c]
        m["pos"] = pos[c]
        in_maps.append(m)
    res = run_bass_kernel_spmd(nc, in_maps, core_ids=list(range(NCORES)))
    out = np.stack([np.asarray(r["y"]) for r in res.results], axis=0)
    return out.reshape(16, S, D).astype(np.float32)
```
